# Optimizing a Trainium2 kernel written in Bass

```python
import math
import jax, jax.numpy as jnp
from jax import lax
import numpy as np

D_MODEL = 1024
BATCH = 32
SEQ = 2048
DEPTH = 1

CHUNK = 64
N_META = 16
N_HEADS = 8
HEAD_DIM = 64
ROT_DIM = HEAD_DIM // 4
ROPE_THETA = 500000.0
Q_BLOCK = 128
ATTN_W = N_HEADS * 2 * HEAD_DIM
CONV_CH = D_MODEL
CONV_K = 31
N_GROUPS = 4
EXPERTS_PER_GROUP = 8
N_EXPERTS = N_GROUPS * EXPERTS_PER_GROUP
TOP_K = 2
D_EXPERT = D_MODEL // 2
MOE_BLOCK = 512
IN_COLS = 3 * ATTN_W + 2 * CONV_CH + 2 * D_MODEL
EPS = 1e-6

kernel_name = "hybrid_diffattn_conformer_hiermoe"


def rmsnorm(x, g):
    xf = x.astype(jnp.float32)
    y = xf * lax.rsqrt(jnp.mean(xf * xf, axis=-1, keepdims=True) + EPS)
    return (y * g.astype(jnp.float32)).astype(x.dtype)


def layernorm(x, g, b):
    xf = x.astype(jnp.float32)
    mu = jnp.mean(xf, axis=-1, keepdims=True)
    var = jnp.mean(jnp.square(xf - mu), axis=-1, keepdims=True)
    y = (xf - mu) * lax.rsqrt(var + EPS)
    return (y * g.astype(jnp.float32) + b.astype(jnp.float32)).astype(x.dtype)


def chunk_ids(n):
    p = jnp.arange(n)
    return jnp.where(p < N_META, 0, 1 + (p - N_META) // CHUNK)


def partial_rope(x):
    L = x.shape[1]
    half = ROT_DIM // 2
    inv_freq = ROPE_THETA ** (-jnp.arange(0, ROT_DIM, 2, dtype=jnp.float32) / ROT_DIM)
    ang = jnp.arange(L, dtype=jnp.float32)[:, None] * inv_freq[None, :]
    cos = jnp.cos(ang)[:, None, None, :]
    sin = jnp.sin(ang)[:, None, None, :]
    xr = x[..., :ROT_DIM].astype(jnp.float32)
    x1, x2 = xr[..., :half], xr[..., half:]
    rot = jnp.concatenate([x1 * cos - x2 * sin, x2 * cos + x1 * sin], axis=-1)
    return jnp.concatenate([rot.astype(x.dtype), x[..., ROT_DIM:]], axis=-1)


def diff_attention(q, k, v, lam):
    B, L = q.shape[0], q.shape[1]
    Lp = -(-L // Q_BLOCK) * Q_BLOCK
    pad = Lp - L
    q = jnp.pad(q, ((0, 0), (0, pad), (0, 0), (0, 0), (0, 0)))
    k = jnp.pad(k, ((0, 0), (0, pad), (0, 0), (0, 0), (0, 0)))
    v = jnp.pad(v, ((0, 0), (0, pad), (0, 0), (0, 0)))
    cid = chunk_ids(Lp)
    nb = Lp // Q_BLOCK
    qb = q.reshape(B, nb, Q_BLOCK, N_HEADS, 2, HEAD_DIM).transpose(1, 0, 2, 3, 4, 5)
    cq = cid.reshape(nb, Q_BLOCK)
    scale = HEAD_DIM ** -0.5

    def one_block(args):
        qi, ci = args
        s = jnp.einsum('bqhmd,bkhmd->bhmqk', qi, k,
                       preferred_element_type=jnp.float32) * scale
        mask = cid[None, :] <= ci[:, None]
        s = jnp.where(mask, s, -jnp.inf)
        p = jax.nn.softmax(s, axis=-1)
        a = p[:, :, 0] - lam * p[:, :, 1]
        return jnp.einsum('bhqk,bkhe->bqhe', a.astype(v.dtype), v)

    o = lax.map(one_block, (qb, cq))
    o = o.transpose(1, 0, 2, 3, 4).reshape(B, Lp, N_HEADS, 2 * HEAD_DIM)
    return o[:, :L]


def conformer_conv(u, conv_w, conv_b, ln_g, ln_b, w_pw2):
    a, b = jnp.split(u, 2, axis=-1)
    z = a * jax.nn.sigmoid(b)
    z = lax.conv_general_dilated(z, conv_w[:, None, :].astype(z.dtype), window_strides=(1,),
                                 padding=[(CONV_K - 1, 0)],
                                 dimension_numbers=('NWC', 'WIO', 'NWC'),
                                 feature_group_count=CONV_CH)
    z = z + conv_b
    z = layernorm(z, ln_g, ln_b)
    z = jax.nn.silu(z)
    return z @ w_pw2


def hier_moe(h, w_group, b_group, w_router, b_router, w_gate, w_up, w_down):
    Bn, L, D = h.shape
    N = Bn * L
    hf = h.reshape(N, D)
    hf32 = hf.astype(jnp.float32)
    g_prob = jax.nn.softmax(hf32 @ w_group.astype(jnp.float32) + b_group.astype(jnp.float32), axis=-1)
    g_w, g_idx = lax.top_k(g_prob, 1)
    e_logits = (hf32 @ w_router.astype(jnp.float32) + b_router.astype(jnp.float32))
    e_logits = e_logits.reshape(N, N_GROUPS, EXPERTS_PER_GROUP)
    e_in = jnp.take_along_axis(e_logits, g_idx[:, :, None], axis=1)[:, 0]
    e_w, e_loc = lax.top_k(jax.nn.softmax(e_in, axis=-1), TOP_K)
    e_w = e_w / jnp.sum(e_w, axis=-1, keepdims=True)
    gate = g_w * e_w
    eid = g_idx * EXPERTS_PER_GROUP + e_loc
    A = N * TOP_K
    flat_e = eid.reshape(A)
    flat_t = jnp.repeat(jnp.arange(N, dtype=jnp.int32), TOP_K)
    flat_w = gate.reshape(A)
    order = jnp.argsort(flat_e)
    se, st, sw = flat_e[order], flat_t[order], flat_w[order]
    counts = jnp.bincount(flat_e, length=N_EXPERTS)
    starts = jnp.cumsum(counts) - counts
    pcounts = (counts + MOE_BLOCK - 1) // MOE_BLOCK * MOE_BLOCK
    pends = jnp.cumsum(pcounts)
    pstarts = pends - pcounts
    dest = pstarts[se] + jnp.arange(A) - starts[se]
    nb = -(-A // MOE_BLOCK) + N_EXPERTS
    P = nb * MOE_BLOCK
    buf_t = jnp.full((P,), N, dtype=jnp.int32).at[dest].set(st)
    buf_w = jnp.zeros((P,), dtype=h.dtype).at[dest].set(sw.astype(h.dtype))
    blk_e = jnp.minimum(jnp.searchsorted(pends, jnp.arange(nb) * MOE_BLOCK, side='right'),
                        N_EXPERTS - 1)
    h_pad = jnp.concatenate([hf, jnp.zeros((1, D), hf.dtype)], axis=0)

    def expert_block(args):
        tok, e = args
        xb = h_pad[tok]
        return (jax.nn.silu(xb @ w_gate[e]) * (xb @ w_up[e])) @ w_down[e]

    y = lax.map(expert_block, (buf_t.reshape(nb, MOE_BLOCK), blk_e)).reshape(P, D)
    out = jnp.zeros((N + 1, D), h.dtype).at[buf_t].add(y * buf_w[:, None])[:N]
    return out.reshape(Bn, L, D)


def setup_inputs(seed: int = 0) -> dict:
    key = jax.random.key(seed)
    ks = jax.random.split(key, 32)
    f32 = jnp.float32
    nrm = lambda k, shape, s: jax.random.normal(k, shape, f32) * s
    Dd = DEPTH
    return {
        "x": nrm(ks[0], (BATCH, SEQ, D_MODEL), 1.0),
        "meta_tokens": nrm(ks[1], (N_META, D_MODEL), 1.0),
        "norm1_g": 1.0 + nrm(ks[2], (Dd, D_MODEL), 0.01),
        "w_in": nrm(ks[3], (Dd, D_MODEL, IN_COLS), D_MODEL ** -0.5),
        "lam_q1": nrm(ks[4], (Dd, HEAD_DIM), 0.1),
        "lam_k1": nrm(ks[5], (Dd, HEAD_DIM), 0.1),
        "lam_q2": nrm(ks[6], (Dd, HEAD_DIM), 0.1),
        "lam_k2": nrm(ks[7], (Dd, HEAD_DIM), 0.1),
        "subln_g": 1.0 + nrm(ks[8], (Dd, 2 * HEAD_DIM), 0.01),
        "w_o_attn": nrm(ks[9], (Dd, ATTN_W, D_MODEL), ATTN_W ** -0.5),
        "conv_w": nrm(ks[10], (Dd, CONV_K, CONV_CH), CONV_K ** -0.5),
        "conv_b": nrm(ks[11], (Dd, CONV_CH), 0.01),
        "conv_ln_g": 1.0 + nrm(ks[12], (Dd, CONV_CH), 0.01),
        "conv_ln_b": nrm(ks[13], (Dd, CONV_CH), 0.01),
        "w_pw2": nrm(ks[14], (Dd, CONV_CH, D_MODEL), CONV_CH ** -0.5),
        "w_out": nrm(ks[15], (Dd, D_MODEL, D_MODEL), D_MODEL ** -0.5),
        "norm2_g": 1.0 + nrm(ks[16], (Dd, D_MODEL), 0.01),
        "w_group": nrm(ks[17], (Dd, D_MODEL, N_GROUPS), D_MODEL ** -0.5),
        "b_group": nrm(ks[18], (Dd, N_GROUPS), 0.01),
        "w_router": nrm(ks[19], (Dd, D_MODEL, N_EXPERTS), D_MODEL ** -0.5),
        "b_router": nrm(ks[20], (Dd, N_EXPERTS), 0.01),
        "w_e_gate": nrm(ks[21], (Dd, N_EXPERTS, D_MODEL, D_EXPERT), D_MODEL ** -0.5),
        "w_e_up": nrm(ks[22], (Dd, N_EXPERTS, D_MODEL, D_EXPERT), D_MODEL ** -0.5),
        "w_e_down": nrm(ks[23], (Dd, N_EXPERTS, D_EXPERT, D_MODEL), D_EXPERT ** -0.5),
        "final_g": 1.0 + nrm(ks[24], (D_MODEL,), 0.01),
    }


def reference(x, meta_tokens, norm1_g, w_in, lam_q1, lam_k1, lam_q2, lam_k2, subln_g,
              w_o_attn, conv_w, conv_b, conv_ln_g, conv_ln_b, w_pw2, w_out, norm2_g,
              w_group, b_group, w_router, b_router, w_e_gate, w_e_up, w_e_down, final_g):
    B = x.shape[0]
    meta = jnp.broadcast_to(meta_tokens.astype(x.dtype)[None], (B, N_META, D_MODEL))
    h_res = jnp.concatenate([meta, x], axis=1)
    L = h_res.shape[1]
    for l in range(DEPTH):
        lam_init = 0.8 - 0.6 * math.exp(-0.3 * l)
        h = rmsnorm(h_res, norm1_g[l])
        proj = h @ w_in[l]
        q, k, v, u, gl = jnp.split(
            proj, [ATTN_W, 2 * ATTN_W, 3 * ATTN_W, 3 * ATTN_W + 2 * CONV_CH], axis=-1)
        q = partial_rope(q.reshape(B, L, N_HEADS, 2, HEAD_DIM))
        k = partial_rope(k.reshape(B, L, N_HEADS, 2, HEAD_DIM))
        v = v.reshape(B, L, N_HEADS, 2 * HEAD_DIM)
        lam = (jnp.exp(jnp.sum(lam_q1[l].astype(jnp.float32) * lam_k1[l].astype(jnp.float32)))
               - jnp.exp(jnp.sum(lam_q2[l].astype(jnp.float32) * lam_k2[l].astype(jnp.float32)))
               + lam_init)
        o = diff_attention(q, k, v, lam)
        o = rmsnorm(o, subln_g[l]) * (1.0 - lam_init)
        y_attn = o.reshape(B, L, ATTN_W) @ w_o_attn[l]
        y_conv = conformer_conv(u, conv_w[l], conv_b[l], conv_ln_g[l], conv_ln_b[l], w_pw2[l])
        g_attn, g_conv = jnp.split(jax.nn.sigmoid(gl), 2, axis=-1)
        h_res = h_res + (g_attn * y_attn + g_conv * y_conv) @ w_out[l]
        h2 = rmsnorm(h_res, norm2_g[l])
        h_res = h_res + hier_moe(h2, w_group[l], b_group[l], w_router[l], b_router[l],
                                 w_e_gate[l], w_e_up[l], w_e_down[l])
    y = rmsnorm(h_res, final_g)
    return y[:, N_META:]
```

```python
import math
import numpy as np
import concourse.bass as bass
import concourse.mybir as mybir
from concourse.bass_utils import run_bass_kernel_spmd

F32 = mybir.dt.float32
BF16 = mybir.dt.bfloat16
I32 = mybir.dt.int32
AF = mybir.ActivationFunctionType
ALU = mybir.AluOpType
AX = mybir.AxisListType

D = 1024
NMETA = 16
NH = 8
HD = 64
CONVK = 31
NE = 32
DE = 512
EPS = 1e-6
LAM_INIT = 0.8 - 0.6 * math.exp(-0.3 * 0)
NCORES = 8
TB = 512
NSLAB = 20
RSQ_MAGIC = 1597463007.0


class Buf:
    __slots__ = ("ap", "w", "r", "ex", "const")

    def __init__(self, ap=None, ex=False, const=False):
        self.ap = ap
        self.w = None
        self.r = []
        self.ex = ex
        self.const = const


class Op:
    __slots__ = ("id", "eng", "key", "inc", "fn", "deps", "cost", "lat", "num", "tag", "t0")


class Prog:
    ENG = ("pe", "act", "dve", "pool", "sp")
    DCOST = {"pe": 0.7, "act": 0.35, "dve": 0.25, "pool": 1.5, "sp": 0.15}

    def __init__(self, nc):
        self.nc = nc
        self.ops = []
        self.lo = 0
        self.cnt = {}
        self.waited = {e: {} for e in self.ENG}
        self.semh = {}
        self.ctx = []
        self.last = {}
        self.nosched = False
        self.tag = (0, 0)
        self.role = "front"
        self.split = {}
        self.splitmap = {}

    def sem(self, key):
        if key not in self.semh:
            cm = self.nc.semaphore("s_" + key)
            self.semh[key] = cm.__enter__()
            self.ctx.append(cm)
            self.cnt[key] = 0
        return self.semh[key]

    def _remap(self, bs):
        if not self.split:
            return bs
        out = []
        for b in bs:
            if id(b) in self.split:
                k = (id(b), self.role)
                if k not in self.splitmap:
                    self.splitmap[k] = Buf(ex=b.ex, const=b.const)
                out.append(self.splitmap[k])
            else:
                out.append(b)
        return out

    def _record(self, eng, key, inc, fn, reads, writes, extra, cost, lat):
        self.sem(key)
        reads = self._remap(reads)
        writes = self._remap(writes)
        ops = self.ops
        d = set()
        for b in reads:
            if b.w is not None:
                d.add(b.w)
            if b.ex:
                d.update(i for i in b.r if ops[i].eng != eng)
        for b in writes:
            if b.w is not None:
                d.add(b.w)
            d.update(b.r)
        d.update(t for t in extra if t is not None)
        o = Op()
        o.id = len(ops)
        o.eng = eng
        o.key = key
        o.inc = inc
        o.fn = fn
        o.deps = d
        o.cost = self.DCOST[eng] if cost is None else cost
        o.lat = lat
        o.num = None
        o.tag = self.tag
        o.t0 = 0.0
        ops.append(o)
        for b in writes:
            b.w = o.id
            b.r = []
        for b in reads:
            if not b.const:
                b.r.append(o.id)
        self.last[key] = o.id
        return o.id

    def op(self, eng, fn, reads=(), writes=(), extra=(), cost=None):
        return self._record(eng, eng, 1, fn, reads, writes, extra, cost, 0.15)

    def dma(self, eng, semkey, fn, reads=(), writes=(), extra=(), cost=None, lat=3.0):
        return self._record(eng, semkey, 16, fn, reads, writes, extra, cost, lat)

    def _schedule(self, phase):
        import heapq
        lo = self.lo
        order = {e: [] for e in self.ENG}
        if self.nosched:
            for o in phase:
                order[o.eng].append(o)
            return order
        indeg = {}
        succ = {}
        ready = {}
        for o in phase:
            n = 0
            for dd in o.deps:
                if dd >= lo:
                    n += 1
                    succ.setdefault(dd, []).append(o)
            indeg[o.id] = n
            ready[o.id] = 0.0
        fut = {e: [] for e in self.ENG}
        av = {e: [] for e in self.ENG}
        free = {e: 0.0 for e in self.ENG}
        import os as _os2
        mode = _os2.environ.get("KPRIO", "bl")
        wbl = float(_os2.environ.get("KPRIOW", "1.0"))
        bl = {}
        for o in reversed(phase):
            m_ = 0.0
            for s_ in succ.get(o.id, ()):
                if bl[s_.id] > m_:
                    m_ = bl[s_.id]
            bl[o.id] = o.cost + o.lat + m_
        if mode == "bl":
            def pk(o):
                return (-(bl[o.id] - wbl * 0.0), o.id)
        else:
            def pk(o):
                return (o.id, o.id)
        for o in phase:
            if indeg[o.id] == 0:
                heapq.heappush(av[o.eng], (pk(o), o.id, o))
        left = len(phase)
        while left:
            best = None
            for e in self.ENG:
                f = fut[e]
                while f and f[0][0] <= free[e]:
                    _, oid, oo = heapq.heappop(f)
                    heapq.heappush(av[e], (pk(oo), oid, oo))
                if av[e]:
                    cand = (free[e], av[e][0][1], e, True)
                elif f:
                    cand = (f[0][0], f[0][1], e, False)
                else:
                    continue
                if best is None or cand < best:
                    best = cand
            t, _, e, isav = best
            if isav:
                _, _, o = heapq.heappop(av[e])
            else:
                _, _, o = heapq.heappop(fut[e])
            free[e] = t + o.cost
            o.t0 = t
            fin = t + o.cost + o.lat
            order[e].append(o)
            left -= 1
            for s_ in succ.get(o.id, ()):
                if fin > ready[s_.id]:
                    ready[s_.id] = fin
                indeg[s_.id] -= 1
                if indeg[s_.id] == 0:
                    heapq.heappush(fut[s_.eng], (ready[s_.id], s_.id, s_))
        self.est = max(free.values())
        return order

    def flush(self, final_waits=()):
        nc = self.nc
        semh = self.semh
        if final_waits:
            self._record("sp", "sp", 1, None, (), (), [t for t in final_waits if t is not None], 0.05, 0.0)
        phase = self.ops[self.lo:]
        order = self._schedule(phase)
        self.lo = len(self.ops)
        for e in self.ENG:
            for o in order[e]:
                self.cnt[o.key] += o.inc
                o.num = self.cnt[o.key]
        ops = self.ops
        plan = {}
        for e in self.ENG:
            lst = []
            wd = self.waited[e]
            for o in order[e]:
                best = {}
                for dd in o.deps:
                    p = ops[dd]
                    if p.num > best.get(p.key, 0):
                        best[p.key] = p.num
                wl = []
                for key, val in best.items():
                    if wd.get(key, 0) >= val:
                        continue
                    wd[key] = val
                    wl.append((key, val))
                lst.append((o, wl))
            plan[e] = lst

        def run(e, lst):
            for o, wl in lst:
                for key, val in wl:
                    e.wait_ge(semh[key], val)
                if o.fn is None:
                    continue
                ins = o.fn(e)
                ins.then_inc(semh[o.key], o.inc)

        with nc.Block() as block:
            @block.tensor
            def _(e):
                run(e, plan["pe"])

            @block.scalar
            def _(e):
                run(e, plan["act"])

            @block.vector
            def _(e):
                run(e, plan["dve"])

            @block.gpsimd
            def _(e):
                run(e, plan["pool"])

            @block.sync
            def _(e):
                run(e, plan["sp"])

    def close(self):
        for cm in reversed(self.ctx):
            cm.__exit__(None, None, None)


class _Stop(Exception):
    pass


def build_nc(NSEQ, S, stop_phase1=False, stop_at=None):
    def ckpt(n):
        P.tag = (P.tag[0], n)
        if stop_at is not None and n == stop_at:
            raise _Stop()
    NT = S // 128
    NB = S // TB
    NTOK = NSEQ * S
    GT = min(16, NT)
    NG = (NSEQ * NT) // GT
    KC = S + NMETA

    nc = bass.Bass("TRN2", target_bir_lowering=False)
    dt_in = lambda name, shape: nc.dram_tensor(name, list(shape), F32, kind="ExternalInput").ap()
    x_d = dt_in("x", (NTOK, D))
    meta_d = dt_in("meta", (NMETA, D))
    wcat_d = dt_in("wcat", (D, NSLAB * 512))
    ident_d = dt_in("ident", (128, 128))
    ustr_d = dt_in("ustr", (128, 128))
    gvec_d = dt_in("gvec", (128, 3 * D))
    rope_d = dt_in("rope", (NMETA + S, 16))
    convw_d = dt_in("convw", (D, CONVK))
    cvec_d = dt_in("cvec", (D, 3))
    lamv_d = dt_in("lamv", (128, 4 * HD))
    subg_d = dt_in("subg", (128, 128))
    wr_d = dt_in("wr", (D, 36))
    br_d = dt_in("br", (128, 36))
    weg_f = dt_in("weg", (NE * 256, 2048))
    weu_f = dt_in("weu", (NE * 256, 2048))
    wed_f = dt_in("wed", (NE * 256, 2048))
    y_d = nc.dram_tensor("y", [NTOK, D], F32, kind="ExternalOutput").ap()
    hres_d = y_d if stop_phase1 else nc.dram_tensor("hres", [NTOK, D], F32, kind="Internal").ap()

    P = Prog(nc)
    import os as _os
    if _os.environ.get('KNOSCHED'):
        P.nosched = True
    cms = []

    def sb(name, shape, dt):
        cm = nc.sbuf_tensor("sb_" + name, list(shape), dt)
        t = cm.__enter__()
        cms.append(cm)
        return t

    def free_to(n):
        while len(cms) > n:
            cms.pop().__exit__(None, None, None)

    pcm = [nc.psum_tensor("ps%d" % i, [128, 512], F32) for i in range(8)]
    ps = [c.__enter__() for c in pcm]
    psb = [Buf(t, ex=True) for t in ps]

    ident_f = sb("ident_f", (128, 128), F32)
    ident_b = sb("ident_b", (128, 128), BF16)
    gvec = sb("gvec", (128, D), F32)
    cosr = sb("cosr", (128, NT, 16), F32)
    ropem = sb("ropem", (16, 16), F32)
    convw = sb("convw", (128, 8, CONVK), F32)
    cvec = sb("cvec", (128, 8, 3), F32)
    hgb = sb("hgb", (128, 8, 2), F32)
    lamv = sb("lamv", (128, 4 * HD), F32)
    subg = sb("subg", (128, 128), F32)
    neglam = sb("neglam", (128, 1), F32)
    lamt = sb("lamt", (128, 4), F32)
    wr = sb("wr", (128, 8, 36), F32)
    br = sb("br", (128, 36), F32)
    ones_b = sb("ones_b", (128, 128), BF16)
    junkc = sb("junkc", (128, HD), BF16)
    B_const = Buf(const=True)
    B_junk = Buf()

    def ld(dst, src, key="c_ld"):
        return P.dma("sp", key, lambda e: e.dma_start(out=dst, in_=src), writes=[B_const])

    ld(ident_f[:], ident_d)
    ld(gvec[:], gvec_d[:, 0:D])
    ld(cosr[:], rope_d[NMETA:NMETA + S, :].rearrange("(t p) c -> p t c", p=128))
    ld(ropem[:], rope_d[0:NMETA, :])
    ld(convw[:], convw_d.rearrange("(c p) k -> p c k", p=128))
    ld(cvec[:], cvec_d.rearrange("(c p) k -> p c k", p=128))
    ld(lamv[:], lamv_d)
    ld(subg[:], subg_d)
    ld(wr[:], wr_d.rearrange("(c p) k -> p c k", p=128))
    ld(br[:], br_d)

    def dve(fn, reads=(), writes=(), extra=(), cost=None):
        return P.op("dve", fn, reads, writes, extra, cost)

    def act(fn, reads=(), writes=(), extra=(), cost=None):
        return P.op("act", fn, reads, writes, extra, cost)

    def pe(fn, reads=(), writes=(), extra=(), cost=None):
        return P.op("pe", fn, reads, writes, extra, cost)

    C = [B_const]
    dve(lambda e: e.tensor_copy(out=ident_b[:], in_=ident_f[:]), reads=C, writes=C)
    dve(lambda e: e.memset(ones_b[:], 1.0 / 1024.0), writes=C)
    dve(lambda e: e.tensor_scalar(out=convw[:], in0=convw[:], scalar1=0.5, scalar2=None, op0=ALU.mult),
        reads=C, writes=C)
    dve(lambda e: e.tensor_scalar(out=subg[:], in0=subg[:], scalar1=2.0 * (1.0 - LAM_INIT), scalar2=None,
                                  op0=ALU.mult), reads=C, writes=C)
    dve(lambda e: e.tensor_scalar(out=hgb[:], in0=cvec[:, :, 1:3], scalar1=0.5, scalar2=None, op0=ALU.mult),
        reads=C, writes=C)
    dve(lambda e: e.scalar_tensor_tensor(out=junkc[:, 0:HD], in0=lamv[:, 0:HD], scalar=1.0,
                                         in1=lamv[:, HD:2 * HD], op0=ALU.mult, op1=ALU.mult,
                                         accum_out=lamt[:, 0:1]), reads=C, writes=C)
    dve(lambda e: e.scalar_tensor_tensor(out=junkc[:, 0:HD], in0=lamv[:, 2 * HD:3 * HD], scalar=1.0,
                                         in1=lamv[:, 3 * HD:4 * HD], op0=ALU.mult, op1=ALU.mult,
                                         accum_out=lamt[:, 1:2]), reads=C, writes=C)
    act(lambda e: e.activation(out=lamt[:, 2:4], in_=lamt[:, 0:2], func=AF.Exp), reads=C, writes=C)
    dve(lambda e: e.tensor_tensor(out=neglam[:], in0=lamt[:, 3:4], in1=lamt[:, 2:3], op=ALU.subtract),
        reads=C, writes=C)
    dve(lambda e: e.tensor_scalar(out=neglam[:], in0=neglam[:], scalar1=-LAM_INIT, scalar2=None, op0=ALU.add),
        reads=C, writes=C)

    g1 = gvec[:, 0:D]

    rs_y = sb("rs_y", (128, 8), F32)
    rs_t = sb("rs_t", (128, 8), F32)
    B_rs = Buf()

    def rsqrt(vb, v_ap, out_b, out_ap, pn, shape2, scr=None):
        n = shape2
        if scr is None:
            y = rs_y[:pn, 0:n]
            t = rs_t[:pn, 0:n]
            R = [B_rs]
        else:
            y, t, R = scr
        rc = 0.12 + n / 960.0
        dve(lambda e: e.tensor_scalar(out=t.bitcast(I32), in0=v_ap.bitcast(I32), scalar1=1, scalar2=None,
                                      op0=ALU.arith_shift_right), reads=[vb], writes=R, cost=rc)
        dve(lambda e: e.tensor_scalar(out=y.bitcast(I32), in0=t.bitcast(I32), scalar1=-1.0, scalar2=RSQ_MAGIC,
                                      op0=ALU.mult, op1=ALU.add), reads=R, writes=R, cost=rc)
        for it in range(3):
            dve(lambda e: e.tensor_tensor(out=t, in0=y, in1=y, op=ALU.mult), reads=R, writes=R, cost=rc)
            dve(lambda e: e.tensor_tensor(out=t, in0=t, in1=v_ap, op=ALU.mult), reads=R + [vb], writes=R, cost=rc)
            dve(lambda e: e.tensor_scalar(out=t, in0=t, scalar1=-0.5, scalar2=1.5, op0=ALU.mult, op1=ALU.add),
                reads=R, writes=R, cost=rc)
            if it < 2:
                dve(lambda e: e.tensor_tensor(out=y, in0=y, in1=t, op=ALU.mult), reads=R, writes=R, cost=rc)
            else:
                dve(lambda e: e.tensor_tensor(out=out_ap, in0=y, in1=t, op=ALU.mult), reads=R, writes=[out_b], cost=rc)

    n_const = len(cms)
    try:
        ckpt(0)
    except _Stop:
        P.flush(); free_to(0)
        for c in reversed(pcm):
            c.__exit__(None, None, None)
        P.close()
        return nc

    KT = sb("KT", (128, NH, KC), BF16)
    VA = sb("VA", (128, NT, NH, 130), BF16)
    VM = sb("VM", (16, NH, 130), BF16)
    NRING = int(_os.environ.get("KRING", "2"))
    NXT = int(_os.environ.get("KXT", "1"))
    ring = [sb("ring%d" % i, (128, 8, 512), BF16) for i in range(NRING)]
    hT = sb("hT", (128, 8, TB), BF16)
    QT = sb("QT", (128, NH, 2, TB), BF16)
    z2Ts = [sb("z2T%d" % i, (128, 8, 30 + TB), BF16) for i in range(2)]
    cacc = [sb("cacc%d" % i, (128, TB), F32) for i in range(2)]
    z2m = sb("z2m", (128, 8, 16), BF16)
    zc = sb("zc", (128, 8, TB), BF16)
    onT = sb("onT", (128, NH, TB), BF16)
    sT = onT
    mixT = sb("mixT", (128, 8, TB), BF16)
    thg = sb("thg", (128, 16, TB), BF16)
    xt = [sb("xt%d" % i, (128, D), F32) for i in range(NXT)]
    hb = sb("hb", (128, D), BF16)
    qkt = [sb("qkt0", (128, 512), BF16)]
    rtmp = sb("rtmp", (128, 4, 64), F32)
    pTf = [sb("pTf%d" % i, (128, 512), BF16) for i in range(3)]
    pTd = [sb("pTd%d" % i, (128, 512), BF16) for i in range(2)]
    o_all = sb("o_all", (128, NH, 128), F32)
    on_all = sb("on_all", (128, NH * 128), BF16)
    sm = sb("sm", (128, 32), F32)
    smA = sb("smA", (128, 2, 4), F32)
    smH = sb("smH", (128, NH, 4), F32)
    rsA = sb("rsA", (128, 2, 2), F32)
    rsQ = sb("rsQ", (128, 2, 8), F32)
    B_smA = [Buf(), Buf()]; B_rsA = [Buf(), Buf()]; B_smH = [Buf() for _ in range(NH)]; B_ssH = [Buf() for _ in range(NH)]
    B_rsQ = Buf(); B_oallh = [Buf() for _ in range(NH)]; B_onallh = [Buf() for _ in range(NH)]
    lnm = sb("lnm", (128, TB), F32)
    lnr = sb("lnr", (128, TB), F32)
    ctmp = [sb("ctmp%d" % i, (128, TB), F32) for i in range(2)]
    cbf = [sb("cbf%d" % i, (128, TB), BF16) for i in range(2)]

    B_KT = [Buf() for _ in range(NB * NSEQ + 1)]
    B_VA = [Buf() for _ in range(NB * NSEQ + 1)]
    B_ring = [Buf() for _ in range(NRING)]
    B_hT = Buf(); B_QT = Buf(); B_z2Ts = [Buf(), Buf()]; B_z2m = Buf(); B_cacc = [Buf(), Buf()]
    B_onT = Buf(); B_thg = Buf(); B_mixT = Buf(); B_sT = B_onT
    B_xt = [Buf(), Buf()]; B_hb = Buf(); B_qkt = [Buf(), Buf()]; B_rtmp = Buf()
    B_pTf = [Buf() for _ in range(3)]
    B_pTd = [Buf(), Buf()]
    B_oall = Buf(); B_onall = Buf(); B_sm = Buf(); B_lnm = Buf(); B_lnr = Buf()
    B_ctmp = [Buf(), Buf()]; B_cbf = [Buf(), Buf()]
    B_meta = Buf()

    dve(lambda e: e.memset(VA[:, :, :, 128:130], 1.0), writes=B_VA)
    dve(lambda e: e.memset(QT[:], 0.0), writes=[B_QT], cost=4.0)
    dve(lambda e: e.memset(VM[:, :, 128:130], 1.0), writes=[B_meta])

    state = {"g": 0, "slab": 0, "xs": 0}

    NGB = int(_os.environ.get("KGB", "6"))

    SPLIT = _os.environ.get("KSPLIT", "0") == "1"

    POOLS = _os.environ.get("KPOOLS", "1") == "1"

    def gbank():
        if POOLS:
            if state.get("role", "front") == "front":
                b = state["g"] % 2
                state["g"] += 1
                return b
            b = 2 + (state.setdefault("sp", 0) % 4)
            state["sp"] += 1
            return b
        if SPLIT:
            return 0 if state.get("role", "front") == "front" else 1
        n = 2 if state.get("attn") else NGB
        b = state["g"] % n
        state["g"] += 1
        return b

    def load_slab2(si):
        slot = state["slab"] % NRING
        state["slab"] += 1
        src = wcat_d[:, si * 512:(si + 1) * 512].rearrange("(c p) n -> p c n", p=128)
        b = B_ring[slot]
        t0 = P.dma("pool", "ring%d_0" % slot,
                   lambda e: e.dma_start(out=ring[slot][:, 0:4, :], in_=src[:, 0:4, :]), writes=[b], cost=1.0, lat=8.0)
        b2 = B_ring2[slot]
        t1 = P.dma("pool", "ring%d_1" % slot,
                   lambda e: e.dma_start(out=ring[slot][:, 4:8, :], in_=src[:, 4:8, :]), writes=[b2], cost=1.0, lat=8.0)
        return slot

    B_ring2 = [Buf() for _ in range(NRING)]

    def ring_bufs(slot):
        return [B_ring[slot], B_ring2[slot]]

    def stage_norm(src_ap, n, off):
        xi = state["xs"] % 2
        state["xs"] += 1
        xb = B_xt[xi % NXT]
        xtile = xt[xi % NXT]
        P.dma("sp", "xt%d" % (xi % NXT), lambda e: e.dma_start(out=xtile[:n, :], in_=src_ap), writes=[xb])
        sa = smA[:, xi, :]
        sab = B_smA[xi]
        act(lambda e: e.activation(out=hb[:n, :], in_=xtile[:n, :], func=AF.Square, accum_out=sa[:n, 0:1]),
            reads=[xb], writes=[B_hb, sab], cost=0.95)
        dve(lambda e: e.tensor_scalar(out=sa[:n, 1:2], in0=sa[:n, 0:1], scalar1=1.0 / D, scalar2=EPS,
                                      op0=ALU.mult, op1=ALU.add), reads=[sab], writes=[sab])
        rsqrt(sab, sa[:n, 1:2], sab, sa[:n, 2:3], n, 1, scr=(rsA[:n, xi, 0:1], rsA[:n, xi, 1:2], [B_rsA[xi]]))
        dve(lambda e: e.scalar_tensor_tensor(out=hb[:n, :], in0=xtile[:n, :], scalar=sa[:n, 2:3], in1=g1[:n, :],
                                             op0=ALU.mult, op1=ALU.mult), reads=[xb, sab] + C, writes=[B_hb], cost=1.2)
        bk = gbank()
        pbf = ps[bk][:].bitcast(BF16)

        def tr(e):
            ins = None
            for c in range(8):
                ins = e.transpose(out=pbf[:, c * 128:c * 128 + n], in_=hb[:n, c * 128:(c + 1) * 128],
                                  identity=ident_b[:n, :n])
            return ins
        pe(tr, reads=[B_hb] + C, writes=[psb[bk]], cost=0.5)
        act(lambda e: e.activation(out=hT[:, :, off:off + n],
                                   in_=pbf.rearrange("p (c t) -> p c t", c=8)[:, :, 0:n], func=AF.Copy),
            reads=[psb[bk]], writes=[B_hT], cost=0.9)

    def proj_tm(slot, off, n):
        bk = gbank()

        def mm(e):
            ins = None
            for kc in range(8):
                ins = e.matmul(out=ps[bk][:n, :], lhsT=hT[:, kc, off:off + n], rhs=ring[slot][:, kc, :],
                               start=(kc == 0), stop=(kc == 7))
            return ins
        pe(mm, reads=[B_hT] + ring_bufs(slot), writes=[psb[bk]], cost=2.3)
        return bk

    def rope_evac(bk, n, cs_ap, dst_write, dst_bufs):
        qi = 0
        state["qk"] = state.get("qk", 0) + 1
        qb = B_qkt[qi]
        qk = qkt[qi]
        pv = ps[bk][:n, :].rearrange("p (g d) -> p g d", g=8)
        qv = qk[:n, :].rearrange("p (g d) -> p g d", g=8)
        cosb = cs_ap[:, 0:8].unsqueeze(1).to_broadcast([n, 8, 8])
        sinb = cs_ap[:, 8:16].unsqueeze(1).to_broadcast([n, 8, 8])
        act(lambda e: e.activation(out=qv[:, :, 16:64], in_=pv[:, :, 16:64], func=AF.Copy),
            reads=[psb[bk]], writes=[qb])
        t = [rtmp[:n, j, :].rearrange("p (g d) -> p g d", g=8) for j in range(4)]
        rd = [psb[bk]] + C
        dve(lambda e: e.tensor_tensor(out=t[0], in0=pv[:, :, 0:8], in1=cosb, op=ALU.mult), reads=rd, writes=[B_rtmp])
        dve(lambda e: e.tensor_tensor(out=t[1], in0=pv[:, :, 8:16], in1=sinb, op=ALU.mult), reads=rd, writes=[B_rtmp])
        dve(lambda e: e.tensor_tensor(out=t[2], in0=pv[:, :, 8:16], in1=cosb, op=ALU.mult), reads=rd, writes=[B_rtmp])
        dve(lambda e: e.tensor_tensor(out=t[3], in0=pv[:, :, 0:8], in1=sinb, op=ALU.mult), reads=rd, writes=[B_rtmp])
        dve(lambda e: e.tensor_tensor(out=qv[:, :, 0:8], in0=t[0], in1=t[1], op=ALU.subtract),
            reads=[B_rtmp], writes=[qb])
        dve(lambda e: e.tensor_tensor(out=qv[:, :, 8:16], in0=t[2], in1=t[3], op=ALU.add),
            reads=[B_rtmp], writes=[qb])
        b2 = gbank()
        pbf = ps[b2][:].bitcast(BF16)

        def tr(e):
            ins = None
            for hh in range(4):
                ins = e.transpose(out=pbf[:, hh * 128:hh * 128 + n], in_=qk[:n, hh * 128:(hh + 1) * 128],
                                  identity=ident_b[:n, :n])
            return ins
        pe(tr, reads=[qb] + C, writes=[psb[b2]])
        act(lambda e: dst_write(e, pbf[:, 0:512].rearrange("p (h t) -> p h t", h=4)[:, :, 0:n]),
            reads=[psb[b2]], writes=dst_bufs)

    def proj_fm(slot, cc, ntok):
        bk = gbank()

        def mm(e):
            ins = None
            for kc in range(8):
                ins = e.matmul(out=ps[bk][:, 0:ntok], lhsT=ring[slot][:, kc, cc * 128:(cc + 1) * 128],
                               rhs=hT[:, kc, 0:ntok], start=(kc == 0), stop=(kc == 7))
            return ins
        pe(mm, reads=[B_hT] + ring_bufs(slot), writes=[psb[bk]], cost=8 * (0.04 + ntok / 1950.0))
        return bk

    def glu_pair(slot, q, ntok, dst_ap, dst_b):
        ba = proj_fm(slot, q, ntok)
        if SPLIT:
            act(lambda e: e.activation(out=ctmp[0][:, 0:ntok], in_=ps[ba][:, 0:ntok], func=AF.Copy),
                reads=[psb[ba]], writes=[B_ctmp[0]], cost=0.22 + ntok / 1400.0)
        bb = proj_fm(slot, 2 + q, ntok)
        act(lambda e: e.activation(out=ctmp[1][:, 0:ntok], in_=ps[bb][:, 0:ntok], func=AF.Tanh, scale=0.5),
            reads=[psb[bb]], writes=[B_ctmp[1]], cost=0.22 + ntok / 1400.0)
        if SPLIT:
            dve(lambda e: e.scalar_tensor_tensor(out=dst_ap, in0=ctmp[1][:, 0:ntok], scalar=1.0,
                                                 in1=ctmp[0][:, 0:ntok], op0=ALU.add, op1=ALU.mult),
                reads=[B_ctmp[0], B_ctmp[1]], writes=[dst_b], cost=0.1 + ntok / 960.0)
        else:
            dve(lambda e: e.scalar_tensor_tensor(out=dst_ap, in0=ctmp[1][:, 0:ntok], scalar=1.0,
                                                 in1=ps[ba][:, 0:ntok], op0=ALU.add, op1=ALU.mult),
                reads=[B_ctmp[1], psb[ba]], writes=[dst_b], cost=0.1 + ntok / 960.0)

    def meta_pass():
        ckpt(1)
        stage_norm(meta_d, NMETA, 0)
        ckpt(2)
        for si in (2, 3):
            slot = load_slab2(si)
            bk = proj_tm(slot, 0, NMETA)
            h0 = (si - 2) * 4
            rope_evac(bk, NMETA, ropem[:, :],
                      lambda e, src, h0=h0: e.activation(out=KT[:, h0:h0 + 4, 0:NMETA], in_=src, func=AF.Copy),
                      [B_meta])
        ckpt(3)
        for si in (4, 5):
            slot = load_slab2(si)
            bk = proj_tm(slot, 0, NMETA)
            h0 = (si - 4) * 4
            act(lambda e, bk=bk, h0=h0: e.activation(out=VM[:, h0:h0 + 4, 0:128],
                                                     in_=ps[bk][:NMETA, :].rearrange("p (h d) -> p h d", h=4),
                                                     func=AF.Copy), reads=[psb[bk]], writes=[B_meta])
        ckpt(4)
        for j in range(4):
            slot = load_slab2(6 + j)
            for q in range(2):
                glu_pair(slot, q, NMETA, z2m[:, 2 * j + q, :], B_z2m)
        ckpt(5)

    def attention_qtile(seq, blk, qt, conv_work):
        gi = blk * 4 + qt
        q0 = qt * 128
        kvb = [B_KT[b] for b in range(blk + 1)] + [B_meta]
        vvb = [B_VA[b] for b in range(blk + 1)] + [B_meta]
        for h in range(NH):
            ob = 6 + (state.setdefault("ob", 0) % 2)
            state["ob"] += 1
            units = [("d", gi)]
            j = 0
            while j < gi:
                units.append(("f", j, min(j + 2, gi)))
                j += 2
            first = [True]
            nun = len(units)

            def emit_qk(ui, h=h):
                u = units[ui]
                bank = 2 + (state.setdefault("sp", 0) % 4)
                state["sp"] += 1
                if u[0] == "d":
                    kts = [(16 + gi * 128, 128, 0), (0, 16, 256)]
                else:
                    kts = [(16 + jj * 128, 128, (jj - u[1]) * 256) for jj in range(u[1], u[2])]

                def qk(e, kts=kts, bank=bank, h=h):
                    ins = None
                    for (kc0, nk, co) in kts:
                        ins = e.matmul(out=ps[bank][:nk, co:co + 256].rearrange("p (m q) -> p m q", m=2),
                                       lhsT=KT[:, h, kc0:kc0 + nk], rhs=QT[:, h, :, q0:q0 + 128], start=True, stop=True)
                    return ins
                pe(qk, reads=kvb + [B_QT], writes=[psb[bank]], cost=0.15 * len(kts))
                return bank, kts

            info = emit_qk(0)
            for ui, u in enumerate(units):
                nxt = emit_qk(ui + 1) if ui + 1 < nun else None
                bank, kts = info
                info = nxt
                width = 256 * len(kts)
                S_ = ps[bank]
                if u[0] == "d":
                    pi = state.setdefault("pd", 0) % 2
                    state["pd"] += 1
                    pt = pTd[pi]
                    pb = B_pTd[pi]
                    act(lambda e, pt=pt, S_=S_: e.activation(out=pt[:, 0:256], in_=S_[:, 0:256], func=AF.Exp, scale=0.125),
                        reads=[psb[bank]], writes=[pb], cost=0.4)
                    act(lambda e, pt=pt, S_=S_: e.activation(out=pt[0:16, 256:512], in_=S_[0:16, 256:512], func=AF.Exp,
                                                             scale=0.125), reads=[psb[bank]], writes=[pb], cost=0.4)
                else:
                    pi = state.setdefault("pf", 0) % 3
                    state["pf"] += 1
                    pt = pTf[pi]
                    pb = B_pTf[pi]
                    act(lambda e, pt=pt, S_=S_, width=width: e.activation(out=pt[:, 0:width], in_=S_[:, 0:width],
                                                                         func=AF.Exp, scale=0.125),
                        reads=[psb[bank]], writes=[pb], cost=0.22 + width / 1400.0)
                specs = []
                for m in range(2):
                    for idx, (kc0, nk, co) in enumerate(kts):
                        c0 = co + m * 128
                        last = (ui == nun - 1 and m == 1 and idx == len(kts) - 1)
                        if u[0] == "d" and idx == 0:
                            specs.append((0, 64, 0, 128, c0, VA[0:64, gi, h, 0:129], first[0], False, m))
                            first[0] = False
                            specs.append((64, 128, 64, 128, c0 + 64, VA[64:128, gi, h, 0:129], False, last, m))
                        elif u[0] == "d":
                            specs.append((0, 16, 0, 128, c0, VM[0:16, h, 0:129], first[0], last, m))
                            first[0] = False
                        else:
                            specs.append((0, 128, 0, 128, c0, VA[:, (kc0 - 16) // 128, h, 0:129], first[0], last, m))
                            first[0] = False

                def pv(e, pt=pt, ob=ob, specs=specs):
                    ins = None
                    for (k0, k1, q_0, q_1, c0, rhs, st, sp_, m) in specs:
                        ins = e.matmul(out=ps[ob][q_0:q_1, m * 256:m * 256 + 129], lhsT=pt[k0:k1, c0:c0 + (q_1 - q_0)],
                                       rhs=rhs, start=st, stop=sp_, skip_group_check=True)
                    return ins
                pe(pv, reads=[pb] + vvb, writes=[psb[ob]], cost=0.125 * len(specs))
            O = ps[ob]
            sh = smH[:, h, :]
            shb = B_smH[h]
            dve(lambda e, O=O, sh=sh: e.reciprocal(out=sh[:, 0:1], in_=O[:, 128:129]), reads=[psb[ob]], writes=[shb])
            dve(lambda e, O=O, sh=sh: e.reciprocal(out=sh[:, 1:2], in_=O[:, 384:385]), reads=[psb[ob]], writes=[shb])
            dve(lambda e, sh=sh: e.tensor_tensor(out=sh[:, 2:3], in0=sh[:, 1:2], in1=neglam[:], op=ALU.mult),
                reads=[shb] + C, writes=[shb])
            dve(lambda e, O=O, h=h, sh=sh: e.tensor_scalar(out=o_all[:, h, :], in0=O[:, 0:128], scalar1=sh[:, 0:1],
                                                           scalar2=None, op0=ALU.mult),
                reads=[psb[ob], shb], writes=[B_oallh[h]])
            dve(lambda e, O=O, h=h, sh=sh: e.scalar_tensor_tensor(out=o_all[:, h, :], in0=O[:, 256:384], scalar=sh[:, 2:3],
                                                                  in1=o_all[:, h, :], op0=ALU.mult, op1=ALU.add),
                reads=[psb[ob], shb], writes=[B_oallh[h]])
            dve(lambda e, h=h: e.scalar_tensor_tensor(out=on_all[:, h * 128:(h + 1) * 128], in0=o_all[:, h, :], scalar=1.0,
                                                      in1=o_all[:, h, :], op0=ALU.mult, op1=ALU.mult,
                                                      accum_out=sm[:, 8 + h:9 + h]),
                reads=[B_oallh[h]], writes=[B_onallh[h], B_ssH[h]])
            if conv_work:
                conv_work.pop(0)()
        qp = state.setdefault("qp", 0) % 2
        state["qp"] += 1
        dve(lambda e: e.tensor_scalar(out=sm[:, 16:24], in0=sm[:, 8:16], scalar1=1.0 / 128.0, scalar2=EPS,
                                      op0=ALU.mult, op1=ALU.add), reads=B_ssH, writes=[B_sm])
        rsqrt(B_sm, sm[:, 16:24], B_sm, sm[:, 24:32], 128, 8, scr=(rsQ[:, 0, :], rsQ[:, 1, :], [B_rsQ]))
        dve(lambda e: e.tensor_tensor(out=o_all[:], in0=o_all[:],
                                      in1=sm[:, 24:32].unsqueeze(2).to_broadcast([128, 8, 128]), op=ALU.mult),
            reads=[B_sm] + B_oallh, writes=B_oallh, cost=1.15)
        dve(lambda e: e.tensor_tensor(out=on_all[:].rearrange("p (h d) -> p h d", h=8), in0=o_all[:],
                                      in1=subg[:].unsqueeze(1).to_broadcast([128, 8, 128]), op=ALU.mult),
            reads=B_oallh + C, writes=B_onallh, cost=1.15)
        bk = gbank()
        pbf = ps[bk][:].bitcast(BF16)

        def tr(e):
            ins = None
            for h in range(8):
                ins = e.transpose(out=pbf[:, h * 128:(h + 1) * 128], in_=on_all[:, h * 128:(h + 1) * 128],
                                  identity=ident_b[:])
            return ins
        pe(tr, reads=B_onallh + C, writes=[psb[bk]], cost=0.5)
        act(lambda e: e.activation(out=onT[:, :, q0:q0 + 128], in_=pbf.rearrange("p (h t) -> p h t", h=8),
                                   func=AF.Copy), reads=[psb[bk]], writes=[B_onT], cost=0.95)

    B_S = [[Buf(), Buf()], [Buf(), Buf()]]

    def conv_chunk(cc, z2T, B_z2T):
        def w():
            k = state.setdefault("ca", 0) % 2
            state["ca"] += 1
            acc = cacc[k]
            ab = B_cacc[k]
            dve(lambda e: e.tensor_scalar(out=acc[:], in0=z2T[:, cc, 0:TB], scalar1=convw[:, cc, 0:1],
                                          scalar2=cvec[:, cc, 0:1], op0=ALU.mult, op1=ALU.add),
                reads=[B_z2T] + C, writes=[ab], cost=0.63)
            for j in range(1, CONVK - 1):
                dve(lambda e, j=j: e.scalar_tensor_tensor(out=acc[:], in0=z2T[:, cc, j:j + TB],
                                                          scalar=convw[:, cc, j:j + 1], in1=acc[:],
                                                          op0=ALU.mult, op1=ALU.add),
                    reads=[B_z2T] + C, writes=[ab], cost=0.63)
            j = CONVK - 1
            dve(lambda e: e.scalar_tensor_tensor(out=zc[:, cc, :], in0=z2T[:, cc, j:j + TB],
                                                 scalar=convw[:, cc, j:j + 1], in1=acc[:],
                                                 op0=ALU.mult, op1=ALU.add),
                reads=[B_z2T, ab] + C, writes=[B_zcp[cc]], cost=0.63)
        return w

    B_zcp = [Buf() for _ in range(8)]

    def block(seq, blk, part):
        tok0 = seq * S + blk * TB
        nonlocal B_thg, B_QT, B_hT, B_onT, B_sT, B_mixT, B_zcp
        if _os.environ.get("KFAKE"):
            par = (seq * NB + blk) % 2
            fk = state.setdefault("fk", {})
            if par not in fk:
                fk[par] = dict(thg=Buf(), QT=Buf(), hT=Buf(), onT=Buf(), mixT=Buf(), zcp=[Buf() for _ in range(8)])
            f_ = _os.environ["KFAKE"]
            if "t" in f_: B_thg = fk[par]["thg"]
            if "q" in f_: B_QT = fk[par]["QT"]
            if "h" in f_: B_hT = fk[par]["hT"]
            if "o" in f_: B_onT = fk[par]["onT"]; B_sT = B_onT
            if "m" in f_: B_mixT = fk[par]["mixT"]
            if "z" in f_: B_zcp = fk[par]["zcp"]
        P.tag = (seq * NB + blk + 1, 5)
        state["role"] = "front" if part in ("f1", "f2") else "back"
        P.role = state["role"]
        state["attn"] = (part == "attn")
        zi = (seq * NB + blk) % 2
        z2T = z2Ts[zi]
        B_z2T = B_z2Ts[zi]
        z2Tp = z2Ts[1 - zi]
        B_z2Tp = B_z2Ts[1 - zi]
        kb = B_KT[blk]
        vb = B_VA[blk]
        if part == "f1":
            for t in range(4):
                stage_norm(x_d[tok0 + t * 128:tok0 + (t + 1) * 128, :], 128, t * 128)
            ckpt(6)
        def stage_b(sis):
            for si in sis:
                slot = load_slab2(si)
                for t in range(4):
                    bk = proj_tm(slot, t * 128, 128)
                    gt = blk * 4 + t
                    if si < 2:
                        h0 = si * 4
                        def qdst(e, src, h0=h0, t=t):
                            e.activation(out=QT[0:64, h0:h0 + 4, 0, t * 128:(t + 1) * 128], in_=src[0:64], func=AF.Copy)
                            return e.activation(out=QT[64:128, h0:h0 + 4, 1, t * 128:(t + 1) * 128], in_=src[64:128],
                                                func=AF.Copy)
                        rope_evac(bk, 128, cosr[:, gt, :], qdst, [B_QT])
                    elif si < 4:
                        h0 = (si - 2) * 4
                        c0 = NMETA + gt * 128
                        rope_evac(bk, 128, cosr[:, gt, :],
                                  lambda e, src, h0=h0, c0=c0: e.activation(out=KT[:, h0:h0 + 4, c0:c0 + 128],
                                                                            in_=src, func=AF.Copy), [kb])
                    else:
                        h0 = (si - 4) * 4
                        act(lambda e, bk=bk, h0=h0, gt=gt: e.activation(
                            out=VA[:, gt, h0:h0 + 4, 0:128], in_=ps[bk][:, :].rearrange("p (h d) -> p h d", h=4),
                            func=AF.Copy), reads=[psb[bk]], writes=[vb])

        if part == "f1":
            stage_b((2, 3, 4, 5))
            ckpt(7)
            if blk == 0:
                dve(lambda e: e.memset(z2T[:, :, 0:14], 0.0), writes=[B_z2T])
                dve(lambda e: e.tensor_copy(out=z2T[:, :, 14:30], in_=z2m[:]), reads=[B_z2m], writes=[B_z2T])
            else:
                dve(lambda e: e.tensor_copy(out=z2T[:, :, 0:30], in_=z2Tp[:, :, TB:TB + 30]), reads=[B_z2Tp], writes=[B_z2T])
            for j in range(4):
                slot = load_slab2(6 + j)
                for q in range(2):
                    glu_pair(slot, q, TB, z2T[:, 2 * j + q, 30:30 + TB], B_z2T)
        if part == "f2":
            P.tag = (P.tag[0], 6)
            stage_b((0, 1))
            ckpt(9)
            for j in range(4):
                slot = load_slab2(10 + j)
                for cc in range(4):
                    bk = proj_fm(slot, cc, TB)
                    act(lambda e, bk=bk, idx=j * 4 + cc: e.activation(out=thg[:, idx, :], in_=ps[bk][:, :], func=AF.Tanh,
                                                                      scale=0.5), reads=[psb[bk]], writes=[B_thg], cost=0.6)
        if part == "attn":
            ckpt(8)
            conv_work = [conv_chunk(cc, z2T, B_z2T) for cc in range(8)]
            state["attn"] = True
            state["role"] = "back"
            P.role = "back"
            for qt in range(4):
                cw = []
                for h in range(NH):
                    cw.append(conv_work.pop(0) if (h % 4 == 1 and conv_work) else (lambda: None))
                attention_qtile(seq, blk, qt, cw)
            state["attn"] = False
            while conv_work:
                conv_work.pop(0)()
        if part == "back":
            ckpt(10)
            for j in range(2):
                slot = load_slab2(14 + j)
                for cc in range(4):
                    dc = j * 4 + cc
                    bk = gbank()

                    def mm(e, slot=slot, cc=cc, bk=bk):
                        ins = None
                        for h in range(8):
                            ins = e.matmul(out=ps[bk][:, :], lhsT=ring[slot][:, h, cc * 128:(cc + 1) * 128],
                                           rhs=onT[:, h, :], start=(h == 0), stop=(h == 7))
                        return ins
                    pe(mm, reads=[B_onT] + ring_bufs(slot), writes=[psb[bk]], cost=2.3)
                    dve(lambda e, dc=dc, bk=bk: e.scalar_tensor_tensor(out=mixT[:, dc, :], in0=thg[:, dc, :], scalar=1.0,
                                                                      in1=ps[bk][:, :], op0=ALU.add, op1=ALU.mult),
                        reads=[B_thg, psb[bk]], writes=[B_mixT], cost=0.63)
            ckpt(11)
            b_mean = gbank()
            for cc in range(8):
                pe(lambda e, cc=cc: e.matmul(out=ps[b_mean][:, :], lhsT=ones_b[:], rhs=zc[:, cc, :],
                                             start=(cc == 0), stop=(cc == 7)),
                   reads=[B_zcp[cc]] + C, writes=[psb[b_mean]], cost=0.25)
            dve(lambda e: e.tensor_copy(out=lnm[:], in_=ps[b_mean][:, :]), reads=[psb[b_mean]], writes=[B_lnm], cost=0.63)
            b_msq = gbank()
            state.setdefault("cb", 0)
            for cc in range(8):
                ci2 = state["cb"] % 2
                state["cb"] += 1
                act(lambda e, cc=cc, ci2=ci2: e.activation(out=cbf[ci2][:], in_=zc[:, cc, :], func=AF.Square),
                    reads=[B_zcp[cc]], writes=[B_cbf[ci2]], cost=0.6)
                pe(lambda e, cc=cc, ci2=ci2: e.matmul(out=ps[b_msq][:, :], lhsT=ones_b[:], rhs=cbf[ci2][:],
                                                      start=(cc == 0), stop=(cc == 7)),
                   reads=[B_cbf[ci2]] + C, writes=[psb[b_msq]], cost=0.25)
            dve(lambda e: e.tensor_tensor(out=lnr[:], in0=lnm[:], in1=lnm[:], op=ALU.mult), reads=[B_lnm], writes=[B_lnr])
            dve(lambda e: e.tensor_tensor(out=lnr[:], in0=ps[b_msq][:, :], in1=lnr[:], op=ALU.subtract),
                reads=[psb[b_msq], B_lnr], writes=[B_lnr])
            dve(lambda e: e.tensor_scalar(out=lnr[:], in0=lnr[:], scalar1=EPS, scalar2=None, op0=ALU.add),
                reads=[B_lnr], writes=[B_lnr])
            rsqrt(B_lnr, lnr[:], B_lnr, lnr[:], 128, TB, scr=(ctmp[0][:], ctmp[1][:], [B_ctmp[0], B_ctmp[1]]))
            for cc in range(8):
                ci = state.setdefault("ct", 0) % 2
                state["ct"] += 1
                cj = 1 - ci
                dve(lambda e, cc=cc, ci=ci: e.tensor_tensor(out=ctmp[ci][:], in0=zc[:, cc, :], in1=lnm[:], op=ALU.subtract),
                    reads=[B_zcp[cc], B_lnm], writes=[B_ctmp[ci]], cost=0.63)
                dve(lambda e, ci=ci: e.tensor_tensor(out=ctmp[ci][:], in0=ctmp[ci][:], in1=lnr[:], op=ALU.mult),
                    reads=[B_lnr], writes=[B_ctmp[ci]], cost=0.63)
                act(lambda e, cc=cc, ci=ci: e.activation(out=cbf[ci][:], in_=ctmp[ci][:], func=AF.Tanh,
                                                         scale=hgb[:, cc, 0:1], bias=hgb[:, cc, 1:2]),
                    reads=[B_ctmp[ci]] + C, writes=[B_cbf[ci]], cost=0.6)
                dve(lambda e, cc=cc, ci=ci: e.tensor_scalar(out=ctmp[ci][:], in0=ctmp[ci][:], scalar1=cvec[:, cc, 1:2],
                                                            scalar2=cvec[:, cc, 2:3], op0=ALU.mult, op1=ALU.add),
                    reads=C, writes=[B_ctmp[ci]], extra=[B_cbf[ci].w], cost=0.63)
                dve(lambda e, cc=cc, ci=ci: e.scalar_tensor_tensor(out=sT[:, cc, :], in0=cbf[ci][:], scalar=1.0,
                                                                  in1=ctmp[ci][:], op0=ALU.add, op1=ALU.mult),
                    reads=[B_cbf[ci], B_ctmp[ci]], writes=[B_sT], cost=0.63)
            ckpt(12)
            for j in range(2):
                slot = load_slab2(16 + j)
                for cc in range(4):
                    dc = j * 4 + cc
                    bk = gbank()

                    def mm(e, slot=slot, cc=cc, bk=bk):
                        ins = None
                        for c in range(8):
                            ins = e.matmul(out=ps[bk][:, :], lhsT=ring[slot][:, c, cc * 128:(cc + 1) * 128],
                                           rhs=sT[:, c, :], start=(c == 0), stop=(c == 7))
                        return ins
                    pe(mm, reads=[B_sT] + ring_bufs(slot), writes=[psb[bk]], cost=2.3)
                    ci = state["ct"] % 2
                    state["ct"] += 1
                    dve(lambda e, dc=dc, bk=bk, ci=ci: e.scalar_tensor_tensor(
                        out=ctmp[ci][:], in0=thg[:, 8 + dc, :], scalar=1.0, in1=ps[bk][:, :], op0=ALU.add, op1=ALU.mult),
                        reads=[B_thg, psb[bk]], writes=[B_ctmp[ci]], cost=0.63)
                    dve(lambda e, dc=dc, ci=ci: e.tensor_tensor(out=mixT[:, dc, :], in0=mixT[:, dc, :], in1=ctmp[ci][:],
                                                                op=ALU.add), reads=[B_ctmp[ci]], writes=[B_mixT], cost=0.63)
            ckpt(13)
            slots = [load_slab2(18), load_slab2(19)]
            xh = (lnm, lnr)
            xhb = (B_lnm, B_lnr)
            for t in range(4):
                r0 = tok0 + t * 128
                for j in range(2):
                    P.dma("sp", "xr%d" % j, lambda e, j=j, r0=r0: e.dma_start(out=xh[j][:, :], in_=x_d[r0:r0 + 128, j * 512:(j + 1) * 512]),
                          writes=[xhb[j]])
                    bk = gbank()

                    def mm(e, slot=slots[j], t=t, bk=bk):
                        ins = None
                        for c in range(8):
                            ins = e.matmul(out=ps[bk][:, :], lhsT=mixT[:, c, t * 128:(t + 1) * 128], rhs=ring[slot][:, c, :],
                                           start=(c == 0), stop=(c == 7))
                        return ins
                    pe(mm, reads=[B_mixT] + ring_bufs(slots[j]), writes=[psb[bk]], cost=2.3)
                    dve(lambda e, bk=bk, j=j: e.scalar_tensor_tensor(
                        out=xh[j][:, :], in0=ps[bk][:, :], scalar=0.25, in1=xh[j][:, :], op0=ALU.mult, op1=ALU.add),
                        reads=[psb[bk]], writes=[xhb[j]], cost=0.63)
                    P.dma("sp", "hr%d" % j, lambda e, j=j, r0=r0: e.dma_start(out=hres_d[r0:r0 + 128, j * 512:(j + 1) * 512], in_=xh[j][:, :]),
                          reads=[xhb[j]])

    if _os.environ.get("KSPLITBUF"):
        for b_ in [B_sm, B_junk, B_rs, B_rtmp, B_oall] + B_ctmp + B_cbf + (B_ring + B_ring2 if "r" in _os.environ["KSPLITBUF"] else []):
            P.split[id(b_)] = True
    stopped = False
    try:
        meta_pass()
        blks = [(seq, blk) for seq in range(NSEQ) for blk in range(NB)]
        if _os.environ.get("KORDER", "1") == "1":
            block(blks[0][0], blks[0][1], "f1")
            block(blks[0][0], blks[0][1], "f2")
            for g, (seq, blk) in enumerate(blks):
                block(seq, blk, "attn")
                if g + 1 < len(blks):
                    block(blks[g + 1][0], blks[g + 1][1], "f1")
                block(seq, blk, "back")
                if g + 1 < len(blks):
                    block(blks[g + 1][0], blks[g + 1][1], "f2")
        else:
            for seq, blk in blks:
                for part in ("f1", "f2", "attn", "back"):
                    block(seq, blk, part)
    except _Stop:
        stopped = True

    fin = [P.last[k] for k in ("hr0", "hr1") if k in P.last]
    if stop_phase1 or stopped:
        P.flush(final_waits=fin)
        free_to(0)
        for c in reversed(pcm):
            c.__exit__(None, None, None)
        P.close()
        return nc
    P.flush(final_waits=fin)
    free_to(n_const)

    NTA = NSEQ * NT
    BLK = 512
    NBLK = (NTOK * 2) // BLK + NE
    NSLOT = NBLK * BLK
    h2_d = nc.dram_tensor("h2s", [NTOK, D], BF16, kind="Internal").ap()
    ys_d = nc.dram_tensor("ys", [NSLOT, D], BF16, kind="Internal").ap()
    st_d = nc.dram_tensor("slot_tok", [NSLOT, 16], I32, kind="Internal").ap()

    g2gf = sb("g2gf", (128, 2 * D), F32)
    junk = sb("junk", (128, 1024), BF16)
    ustr = sb("ustr", (128, 128), F32)
    ustr_b = sb("ustr_b", (128, 128), BF16)
    one_b = sb("one_b", (128, 128), BF16)
    ld(g2gf[:], gvec_d[:, D:3 * D], key="c_ld2")
    ld(ustr[:], ustr_d, key="c_ld2")
    g2 = g2gf[:, 0:D]
    gf = g2gf[:, D:2 * D]
    dve(lambda e: e.tensor_copy(out=ustr_b[:], in_=ustr[:]), reads=C, writes=C)
    dve(lambda e: e.memset(one_b[:], 1.0), writes=C)

    S1 = sb("S1", (128, NTA, NE), F32)
    S2 = sb("S2", (128, NTA, NE), F32)
    RK = sb("RK", (128, NTA, NE), F32)
    GA = sb("GA", (128, NTA, 2), F32)
    DSf = sb("DSf", (128, NTA, 2), F32)
    DSi = sb("DSi", (128, NTA, 2), I32)
    TOK = sb("TOK", (128, max(NTA, 8), 16), I32)
    cum = sb("cum", (128, NE), F32)
    misc = sb("misc", (128, 8, NE), F32)
    misci = sb("misci", (128, 2, NE), I32)
    BE = sb("BE", (128, NBLK), F32)
    IDX = sb("IDX", (128, NBLK, 2), I32)
    IDf = sb("IDf", (128, NBLK, 2), F32)
    base2 = sb("base2", (128, 2), F32)
    base2i = sb("base2i", (128, 2), I32)
    ZQ = NSLOT * 16 // 128
    ZW = min(ZQ, 1024)
    zt = sb("zt", (128, ZW), I32)
    hx = [sb("hx%d" % i, (128, D), F32) for i in range(4)]
    h2f = sb("h2f", (128, D), F32)
    h2b = [sb("h2b%d" % i, (128, D), BF16) for i in range(2)]
    h2Tf = sb("h2Tf", (128, 8, 128), F32)
    eb = sb("eb", (128, NE), BF16)
    sm2s = sb("sm2s", (128, 4, 96), F32)
    SSA = sb("SSA", (128, NTA), F32)
    RSA = sb("RSA", (128, NTA), F32)
    RSy = sb("RSy", (128, NTA), F32)
    RSt = sb("RSt", (128, NTA), F32)
    B_ssa = [Buf() for _ in range(NTA)]
    B_SSA = Buf(); B_RSA = Buf(); B_RSs = Buf()
    rs2 = sb("rs2", (128, 4, 2), F32)
    B_sm2s = [Buf() for _ in range(4)]
    B_rs2 = [Buf() for _ in range(4)]
    SI = [sb("SI%d" % i, (128, 4, 16), I32) for i in range(2)]
    XG = [sb("XG%d" % i, (128, 4, D), BF16) for i in range(2)]
    XT = [sb("XT%d" % i, (128, 8, BLK), BF16) for i in range(2)]
    WG = [sb("WG%d" % i, (128, 8, DE), BF16) for i in range(2)]
    WU = [sb("WU%d" % i, (128, 8, DE), BF16) for i in range(2)]
    WD = [sb("WD%d" % i, (128, 4, D), BF16) for i in range(2)]
    hid = [sb("hid%d" % i, (128, 4, BLK), BF16) for i in range(2)]
    slt = [sb("slt%d" % i, (128, BLK), F32) for i in range(2)]
    YB = [sb("YB%d" % i, (128, D), BF16) for i in range(4)]
    Y1 = [sb("Y1_%d" % i, (128, D), BF16) for i in range(2)]
    Y2 = [sb("Y2_%d" % i, (128, D), BF16) for i in range(2)]
    yo = [sb("yo%d" % i, (128, D), F32) for i in range(2)]
    B_S1 = Buf(); B_S2 = Buf(); B_RK = Buf(); B_GA = Buf(); B_DS = Buf(); B_TOK = Buf(); B_cum = Buf()
    B_misc = Buf(); B_BE = Buf(); B_ID = Buf(); B_zt = Buf()
    B_hx = [Buf() for _ in range(4)]; B_h2f = Buf(); B_h2b = [Buf(), Buf()]; B_h2Tf = Buf(); B_eb = Buf()
    B_SI = [Buf(), Buf()]; B_XG = [Buf(), Buf()]; B_XT = [Buf(), Buf()]
    B_WG = [Buf(), Buf()]; B_WU = [Buf(), Buf()]; B_WD = [Buf(), Buf()]
    B_hid = [Buf(), Buf()]; B_slt = [Buf(), Buf()]; B_YB = [Buf() for _ in range(4)]
    B_Y1 = [Buf(), Buf()]; B_Y2 = [Buf(), Buf()]; B_yo = [Buf(), Buf()]
    B_h2d = Buf(); B_std = Buf(); B_ysd = Buf()
    st2 = {"g": 0, "slt": 0, "yb": 0, "hx": 0}

    def gb2():
        b = st2["g"] % 8
        st2["g"] += 1
        return b

    def pool(fn, reads=(), writes=(), extra=(), cost=None):
        return P.op("pool", fn, reads, writes, extra, cost)

    IOA = bass.IndirectOffsetOnAxis
    BIG = 1.0e30
    pool(lambda e: e.iota(TOK[:], pattern=[[128, max(NTA, 8)], [0, 16]], base=0, channel_multiplier=1), writes=[B_TOK])
    pool(lambda e: e.iota(base2i[:], pattern=[[1, 2]], base=0, channel_multiplier=2), writes=[B_TOK])
    dve(lambda e: e.tensor_copy(out=base2[:], in_=base2i[:]), reads=[B_TOK], writes=C)
    dve(lambda e: e.memset(zt[:], 0), writes=[B_zt])
    dve(lambda e: e.memset(cum[:], 0.0), writes=[B_cum])
    for z0 in range(0, ZQ, ZW):
        zw = min(ZW, ZQ - z0)
        P.dma("sp", "zt", lambda e, z0=z0, zw=zw: e.dma_start(
            out=st_d.rearrange("(p q) c -> p (q c)", p=128)[:, z0:z0 + zw], in_=zt[:, 0:zw]),
            reads=[B_zt], writes=[B_std])

    def tile2a(t):
        r0 = t * 128
        hi = t % 2
        sm2 = sm2s[:, t % 4, :]
        B_sm2 = B_sm2s[t % 4]
        rscr = (rs2[:, t % 4, 0:1], rs2[:, t % 4, 1:2], [B_rs2[t % 4]])
        hq = t % 4
        P.dma("sp", "hx%d" % hq, lambda e, hq=hq, r0=r0: e.dma_start(out=hx[hq][:], in_=hres_d[r0:r0 + 128, :]),
              writes=[B_hx[hq]])
        dve(lambda e, hq=hq, t=t: e.scalar_tensor_tensor(out=h2f[:], in0=hx[hq][:], scalar=RSA[:, t:t + 1], in1=g2,
                                                         op0=ALU.mult, op1=ALU.mult),
            reads=[B_hx[hq], B_RSA] + C, writes=[B_h2f], cost=1.2)
        act(lambda e, hi=hi: e.activation(out=h2b[hi][:], in_=h2f[:], func=AF.Copy), reads=[B_h2f], writes=[B_h2b[hi]])
        P.dma("sp", "h2b%d" % hi, lambda e, hi=hi, r0=r0: e.dma_start(out=h2_d[r0:r0 + 128, :], in_=h2b[hi][:]),
              reads=[B_h2b[hi]], writes=[])
        bks = [gb2(), gb2()]
        for q in range(2):
            def tr(e, q=q, bk=bks[q]):
                ins = None
                for c in range(4):
                    ins = e.transpose(out=ps[bk][:, c * 128:(c + 1) * 128],
                                      in_=h2f[:, (q * 4 + c) * 128:(q * 4 + c + 1) * 128], identity=ident_f[:])
                return ins
            pe(tr, reads=[B_h2f] + C, writes=[psb[bks[q]]])
            dve(lambda e, q=q, bk=bks[q]: e.tensor_copy(out=h2Tf[:, q * 4:(q + 1) * 4, :],
                                                        in_=ps[bk][:, :].rearrange("p (c t) -> p c t", c=4)),
                reads=[psb[bks[q]]], writes=[B_h2Tf])
        bk = gb2()

        def rmm(e, bk=bk):
            ins = None
            for c in range(8):
                ins = e.matmul(out=ps[bk][:, 0:36], lhsT=h2Tf[:, c, :], rhs=wr[:, c, :], start=(c == 0), stop=(c == 7))
            return ins
        pe(rmm, reads=[B_h2Tf] + C, writes=[psb[bk]])
        S2_ = [B_sm2]
        lg = sm2[:, 8:44]
        dve(lambda e, bk=bk: e.tensor_tensor(out=lg, in0=ps[bk][:, 0:36], in1=br[:], op=ALU.add),
            reads=[psb[bk]] + C, writes=S2_)
        dve(lambda e: e.tensor_reduce(out=sm2[:, 3:4], in_=sm2[:, 8:12], axis=AX.X, op=ALU.max), reads=S2_, writes=S2_)
        dve(lambda e: e.tensor_scalar(out=sm2[:, 4:5], in0=sm2[:, 3:4], scalar1=-1.0, scalar2=None, op0=ALU.mult),
            reads=S2_, writes=S2_)
        act(lambda e: e.activation(out=sm2[:, 44:48], in_=sm2[:, 8:12], func=AF.Exp, bias=sm2[:, 4:5],
                                   accum_out=sm2[:, 5:6]), reads=S2_, writes=S2_)
        dve(lambda e: e.reciprocal(out=sm2[:, 6:7], in_=sm2[:, 5:6]), reads=S2_, writes=S2_)
        dve(lambda e: e.tensor_scalar(out=sm2[:, 44:48], in0=sm2[:, 8:12], scalar1=sm2[:, 3:4], scalar2=None,
                                      op0=ALU.is_equal), reads=S2_, writes=S2_)
        dve(lambda e: e.tensor_scalar(out=sm2[:, 44:48], in0=sm2[:, 44:48], scalar1=BIG, scalar2=-BIG,
                                      op0=ALU.mult, op1=ALU.add), reads=S2_, writes=S2_)
        dve(lambda e: e.tensor_tensor(out=sm2[:, 48:80].rearrange("p (a b) -> p a b", a=4),
                                      in0=sm2[:, 12:44].rearrange("p (a b) -> p a b", a=4),
                                      in1=sm2[:, 44:48].unsqueeze(2).to_broadcast([128, 4, 8]), op=ALU.add),
            reads=S2_, writes=S2_)
        dve(lambda e: e.max(out=sm2[:, 80:88], in_=sm2[:, 48:80]), reads=S2_, writes=S2_)
        dve(lambda e: e.tensor_tensor(out=sm2[:, 88:89], in0=sm2[:, 81:82], in1=sm2[:, 80:81], op=ALU.subtract),
            reads=S2_, writes=S2_)
        act(lambda e: e.activation(out=sm2[:, 89:90], in_=sm2[:, 88:89], func=AF.Exp), reads=S2_, writes=S2_)
        dve(lambda e: e.tensor_scalar(out=sm2[:, 90:91], in0=sm2[:, 89:90], scalar1=1.0, scalar2=None, op0=ALU.add),
            reads=S2_, writes=S2_)
        dve(lambda e: e.reciprocal(out=sm2[:, 91:92], in_=sm2[:, 90:91]), reads=S2_, writes=S2_)
        dve(lambda e, t=t: e.tensor_tensor(out=GA[:, t, 0:1], in0=sm2[:, 91:92], in1=sm2[:, 6:7], op=ALU.mult),
            reads=S2_, writes=[B_GA])
        dve(lambda e, t=t: e.tensor_tensor(out=GA[:, t, 1:2], in0=GA[:, t, 0:1], in1=sm2[:, 89:90], op=ALU.mult),
            reads=S2_, writes=[B_GA])
        dve(lambda e, t=t: e.tensor_scalar(out=S1[:, t, :], in0=sm2[:, 48:80], scalar1=sm2[:, 80:81], scalar2=None,
                                           op0=ALU.is_equal), reads=S2_, writes=[B_S1])
        dve(lambda e, t=t: e.tensor_scalar(out=S2[:, t, :], in0=sm2[:, 48:80], scalar1=sm2[:, 81:82], scalar2=None,
                                           op0=ALU.is_equal), reads=S2_, writes=[B_S2])
        dve(lambda e, t=t: e.tensor_tensor(out=eb[:], in0=S1[:, t, :], in1=S2[:, t, :], op=ALU.add),
            reads=[B_S1, B_S2], writes=[B_eb])
        bk2 = gb2()

        def rkmm(e, bk2=bk2):
            e.matmul(out=ps[bk2][:, 0:NE], lhsT=ustr_b[:], rhs=eb[:], start=True, stop=True)
            return e.matmul(out=ps[bk2][:, 64:64 + NE], lhsT=one_b[:], rhs=eb[:], start=True, stop=True)
        pe(rkmm, reads=[B_eb] + C, writes=[psb[bk2]])
        dve(lambda e, t=t, bk2=bk2: e.tensor_tensor(out=RK[:, t, :], in0=ps[bk2][:, 0:NE], in1=cum[:], op=ALU.add),
            reads=[psb[bk2], B_cum], writes=[B_RK])
        dve(lambda e, bk2=bk2: e.tensor_tensor(out=cum[:], in0=ps[bk2][:, 64:64 + NE], in1=cum[:], op=ALU.add),
            reads=[psb[bk2]], writes=[B_cum])

    for t in range(NTA):
        hq = t % 4
        r0 = t * 128
        P.dma("sp", "hx%d" % hq, lambda e, hq=hq, r0=r0: e.dma_start(out=hx[hq][:], in_=hres_d[r0:r0 + 128, :]),
              writes=[B_hx[hq]])
        act(lambda e, hq=hq, t=t: e.activation(out=junk[:, :], in_=hx[hq][:], func=AF.Square, accum_out=SSA[:, t:t + 1]),
            reads=[B_hx[hq]], writes=[B_junk, B_ssa[t]], cost=0.95)
    dve(lambda e: e.tensor_scalar(out=SSA[:], in0=SSA[:], scalar1=1.0 / D, scalar2=EPS, op0=ALU.mult, op1=ALU.add),
        reads=B_ssa, writes=[B_SSA])
    rsqrt(B_SSA, SSA[:], B_RSA, RSA[:], 128, NTA, scr=(RSy[:], RSt[:], [B_RSs]))
    for t in range(NTA):
        tile2a(t)

    M = [B_misc]
    cnt_i = misci[:, 0, :]
    pc_i = misci[:, 1, :]
    dve(lambda e: e.tensor_copy(out=cnt_i, in_=cum[:]), reads=[B_cum], writes=M)
    dve(lambda e: e.tensor_scalar(out=cnt_i, in0=cnt_i, scalar1=float(BLK - 1), scalar2=None, op0=ALU.add), reads=M, writes=M)
    dve(lambda e: e.tensor_scalar(out=pc_i, in0=cnt_i, scalar1=9, scalar2=None, op0=ALU.arith_shift_right), reads=M, writes=M)
    dve(lambda e: e.tensor_scalar(out=pc_i, in0=pc_i, scalar1=9, scalar2=None, op0=ALU.logical_shift_left), reads=M, writes=M)
    dve(lambda e: e.tensor_copy(out=misc[:, 0, :], in_=pc_i), reads=M, writes=M)
    dve(lambda e: e.memset(misc[:, 1, :], 0.0), writes=M)
    dve(lambda e: e.tensor_tensor_scan(out=misc[:, 2, :], data0=misc[:, 0, :], data1=misc[:, 1, :], initial=0.0,
                                       op0=ALU.add, op1=ALU.add), reads=M, writes=M)
    dve(lambda e: e.tensor_tensor(out=misc[:, 3, :], in0=misc[:, 2, :], in1=misc[:, 0, :], op=ALU.subtract),
        reads=M, writes=M)
    dve(lambda e: e.tensor_tensor(out=RK[:], in0=RK[:], in1=misc[:, 3, :].unsqueeze(1).to_broadcast([128, NTA, NE]),
                                  op=ALU.add), reads=M, writes=[B_RK])
    dve(lambda e: e.tensor_tensor(out=S1[:], in0=S1[:], in1=RK[:], op=ALU.mult), reads=[B_RK], writes=[B_S1])
    dve(lambda e: e.tensor_tensor(out=S2[:], in0=S2[:], in1=RK[:], op=ALU.mult), reads=[B_RK], writes=[B_S2])
    dve(lambda e: e.tensor_reduce(out=DSf[:, :, 0], in_=S1[:], axis=AX.X, op=ALU.add), reads=[B_S1], writes=[B_DS])
    dve(lambda e: e.tensor_reduce(out=DSf[:, :, 1], in_=S2[:], axis=AX.X, op=ALU.add), reads=[B_S2], writes=[B_DS])
    dve(lambda e: e.tensor_copy(out=DSi[:], in_=DSf[:]), reads=[B_DS], writes=[B_DS])
    for b in range(NBLK):
        dve(lambda e, b=b: e.tensor_scalar(out=misc[:, 4, :], in0=misc[:, 2, :], scalar1=float(b * BLK), scalar2=None,
                                           op0=ALU.is_le, op1=ALU.add, accum_out=BE[:, b:b + 1]),
            reads=M, writes=M + [B_BE])
    dve(lambda e: e.tensor_scalar(out=BE[:], in0=BE[:], scalar1=float(NE - 1), scalar2=None, op0=ALU.min),
        reads=[B_BE], writes=[B_BE])
    dve(lambda e: e.tensor_scalar(out=IDf[:], in0=BE[:].unsqueeze(2).to_broadcast([128, NBLK, 2]), scalar1=256.0,
                                  scalar2=None, op0=ALU.mult), reads=[B_BE], writes=[B_ID])
    dve(lambda e: e.tensor_tensor(out=IDf[:], in0=IDf[:], in1=base2[:].unsqueeze(1).to_broadcast([128, NBLK, 2]),
                                  op=ALU.add), reads=C, writes=[B_ID])
    dve(lambda e: e.tensor_copy(out=IDX[:], in_=IDf[:]), reads=[B_ID], writes=[B_ID])
    sc_ids = []
    for t in range(NTA):
        for k in range(2):
            sc_ids.append(None)
            sc_ids[-1] = P.dma("pool", "sc%d" % ((2 * t + k) % 8),
                  lambda e, t=t, k=k: e.indirect_dma_start(out=st_d, out_offset=IOA(ap=DSi[:, t, k:k + 1], axis=0),
                                                           in_=TOK[:, t, :], in_offset=None),
                  reads=[B_DS, B_TOK, B_std])
    sc_done = list(sc_ids)
    h2_done = [P.last["h2b%d" % i] for i in range(2)]

    for b in range(NBLK):
        wi = b % 2
        P.dma("sp", "si%d" % wi, lambda e, b=b, wi=wi: e.dma_start(
            out=SI[wi][:], in_=st_d[b * BLK:(b + 1) * BLK, :].rearrange("(j p) c -> p j c", p=128)),
            writes=[B_SI[wi]], extra=sc_done)
        for j in range(4):
            P.dma("pool", "xg%d_%d" % (wi, j), lambda e, wi=wi, j=j: e.indirect_dma_start(
                out=XG[wi][:, j, :], out_offset=None, in_=h2_d, in_offset=IOA(ap=SI[wi][:, j, 0:1], axis=0)),
                reads=[B_SI[wi]], writes=[B_XG[wi]] if j == 0 else [], extra=h2_done + ([B_XG[wi].w] if j else []))
        xg_tok = [P.last["xg%d_%d" % (wi, j)] for j in range(4)]
        fw = {}
        for h_ in range(2):
            for nm, Wt, src, Bw in (("wg", WG, weg_f, B_WG), ("wu", WU, weu_f, B_WU), ("wd", WD, wed_f, B_WD)):
                nc_ = Wt[wi].shape[1] // 2
                t_ = P.dma("pool", "%s%d_%d" % (nm, wi, h_), lambda e, Wt=Wt, src=src, wi=wi, h_=h_, b=b, nc_=nc_:
                           e.indirect_dma_start(out=Wt[wi][:, h_ * nc_:(h_ + 1) * nc_, :].rearrange("p c n -> p (c n)"),
                                                out_offset=None, in_=src, in_offset=IOA(ap=IDX[:, b, h_:h_ + 1], axis=0)),
                           reads=[B_ID], writes=[Bw[wi]] if h_ == 0 else [], extra=[fw.get(nm)], cost=1.5, lat=10.0)
                fw.setdefault(nm, t_)
        wg_tok = [P.last["wg%d_%d" % (wi, c)] for c in range(2)]
        wu_tok = [P.last["wu%d_%d" % (wi, c)] for c in range(2)]
        wd_tok = [P.last["wd%d_%d" % (wi, c)] for c in range(2)]
        for j in range(4):
            bk = gb2()
            pbf = ps[bk][:].bitcast(BF16)

            def tr(e, j=j, pbf=pbf, wi=wi):
                ins = None
                for c in range(8):
                    ins = e.transpose(out=pbf[:, c * 128:(c + 1) * 128], in_=XG[wi][:, j, c * 128:(c + 1) * 128],
                                      identity=ident_b[:])
                return ins
            pe(tr, reads=[B_XG[wi]] + C, writes=[psb[bk]], extra=xg_tok)
            act(lambda e, j=j, pbf=pbf, wi=wi: e.activation(out=XT[wi][:, :, j * 128:(j + 1) * 128],
                                                          in_=pbf.rearrange("p (c t) -> p c t", c=8), func=AF.Copy),
                reads=[psb[bk]], writes=[B_XT[wi]])
        hi = b % 2
        for hc in range(4):
            bg = gb2()
            bu = gb2()

            def mmg(e, W=WG[wi], bk=bg, hc=hc, wi=wi):
                ins = None
                for c in range(8):
                    ins = e.matmul(out=ps[bk][:, :], lhsT=W[:, c, hc * 128:(hc + 1) * 128], rhs=XT[wi][:, c, :],
                                   start=(c == 0), stop=(c == 7))
                return ins
            pe(mmg, reads=[B_XT[wi], B_WG[wi]], writes=[psb[bg]], extra=wg_tok, cost=2.3)

            def mmu(e, W=WU[wi], bk=bu, hc=hc, wi=wi):
                ins = None
                for c in range(8):
                    ins = e.matmul(out=ps[bk][:, :], lhsT=W[:, c, hc * 128:(hc + 1) * 128], rhs=XT[wi][:, c, :],
                                   start=(c == 0), stop=(c == 7))
                return ins
            pe(mmu, reads=[B_XT[wi], B_WU[wi]], writes=[psb[bu]], extra=wu_tok, cost=2.3)
            si_ = st2["slt"] % 2
            st2["slt"] += 1
            act(lambda e, bg=bg, si_=si_: e.activation(out=slt[si_][:], in_=ps[bg][:, :], func=AF.Silu),
                reads=[psb[bg]], writes=[B_slt[si_]], cost=0.6)
            dve(lambda e, bu=bu, si_=si_, hi=hi, hc=hc: e.tensor_tensor(
                out=hid[hi][:, hc, :], in0=slt[si_][:], in1=ps[bu][:, :], op=ALU.mult),
                reads=[B_slt[si_], psb[bu]], writes=[B_hid[hi]], cost=0.63)
        for tt in range(4):
            yi = st2["yb"] % 4
            st2["yb"] += 1
            for j in range(2):
                bk = gb2()

                def mmd(e, W=WD[wi], bk=bk, tt=tt, j=j, hi=hi):
                    ins = None
                    for c in range(4):
                        ins = e.matmul(out=ps[bk][:, :], lhsT=hid[hi][:, c, tt * 128:(tt + 1) * 128],
                                       rhs=W[:, c, j * 512:(j + 1) * 512], start=(c == 0), stop=(c == 3))
                    return ins
                pe(mmd, reads=[B_hid[hi], B_WD[wi]], writes=[psb[bk]], extra=wd_tok, cost=1.15)
                if j == 0:
                    act(lambda e, bk=bk, yi=yi: e.activation(out=YB[yi][:, 0:512], in_=ps[bk][:, :], func=AF.Copy),
                        reads=[psb[bk]], writes=[B_YB[yi]])
                else:
                    dve(lambda e, bk=bk, yi=yi: e.tensor_copy(out=YB[yi][:, 512:1024], in_=ps[bk][:, :]),
                        reads=[psb[bk]], writes=[B_YB[yi]])
            s0 = b * BLK + tt * 128
            P.dma("sp", "yb%d" % yi, lambda e, yi=yi, s0=s0: e.dma_start(out=ys_d[s0:s0 + 128, :], in_=YB[yi][:]),
                  reads=[B_YB[yi]])
    ys_done = [P.last["yb%d" % i] for i in range(4)]

    def tile2c(t):
        r0 = t * 128
        hi = t % 2
        hq = t % 4
        sm2 = sm2s[:, t % 4, :]
        B_sm2 = B_sm2s[t % 4]
        rscr = (rs2[:, t % 4, 0:1], rs2[:, t % 4, 1:2], [B_rs2[t % 4]])
        P.dma("sp", "hx%d" % hq, lambda e, hq=hq, r0=r0: e.dma_start(out=hx[hq][:], in_=hres_d[r0:r0 + 128, :]),
              writes=[B_hx[hq]])
        P.dma("pool", "y1_%d" % hi, lambda e, hi=hi, t=t: e.indirect_dma_start(
            out=Y1[hi][:], out_offset=None, in_=ys_d, in_offset=IOA(ap=DSi[:, t, 0:1], axis=0)),
            reads=[B_DS], writes=[B_Y1[hi]], extra=ys_done)
        P.dma("pool", "y2_%d" % hi, lambda e, hi=hi, t=t: e.indirect_dma_start(
            out=Y2[hi][:], out_offset=None, in_=ys_d, in_offset=IOA(ap=DSi[:, t, 1:2], axis=0)),
            reads=[B_DS], writes=[B_Y2[hi]], extra=ys_done)
        dve(lambda e, hi=hi, hq=hq, t=t: e.scalar_tensor_tensor(out=hx[hq][:], in0=Y1[hi][:], scalar=GA[:, t, 0:1], in1=hx[hq][:],
                                                         op0=ALU.mult, op1=ALU.add),
            reads=[B_Y1[hi], B_GA], writes=[B_hx[hq]])
        dve(lambda e, hi=hi, hq=hq, t=t: e.scalar_tensor_tensor(out=hx[hq][:], in0=Y2[hi][:], scalar=GA[:, t, 1:2], in1=hx[hq][:],
                                                         op0=ALU.mult, op1=ALU.add),
            reads=[B_Y2[hi], B_GA], writes=[B_hx[hq]])
        act(lambda e, hq=hq: e.activation(out=junk[:, :], in_=hx[hq][:], func=AF.Square, accum_out=sm2[:, 0:1]),
            reads=[B_hx[hq]], writes=[B_junk, B_sm2])
        dve(lambda e: e.tensor_scalar(out=sm2[:, 1:2], in0=sm2[:, 0:1], scalar1=1.0 / D, scalar2=EPS,
                                      op0=ALU.mult, op1=ALU.add), reads=[B_sm2], writes=[B_sm2])
        rsqrt(B_sm2, sm2[:, 1:2], B_sm2, sm2[:, 2:3], 128, 1, scr=rscr)
        dve(lambda e, hi=hi, hq=hq: e.scalar_tensor_tensor(out=yo[hi][:], in0=hx[hq][:], scalar=sm2[:, 2:3], in1=gf,
                                                    op0=ALU.mult, op1=ALU.mult),
            reads=[B_hx[hq], B_sm2] + C, writes=[B_yo[hi]])
        P.dma("sp", "yo%d" % hi, lambda e, hi=hi, r0=r0: e.dma_start(out=y_d[r0:r0 + 128, :], in_=yo[hi][:]),
              reads=[B_yo[hi]])

    for t in range(NTA):
        tile2c(t)

    fin = [P.last["yo%d" % i] for i in range(2)]
    P.flush(final_waits=fin)
    free_to(0)
    for c in reversed(pcm):
        c.__exit__(None, None, None)
    P.close()
    return nc


def _host_inputs(inputs, NSEQ, S):
    f = lambda a: np.ascontiguousarray(np.asarray(a, dtype=np.float32))
    w_in = f(inputs["w_in"])[0]
    q_, k_, v_ = w_in[:, 0:1024], w_in[:, 1024:2048], w_in[:, 2048:3072]
    ua, ub = w_in[:, 3072:4096], w_in[:, 4096:5120]
    gl = w_in[:, 5120:7168]
    ucat = np.concatenate([np.concatenate([ua[:, 256 * j:256 * (j + 1)], ub[:, 256 * j:256 * (j + 1)]], axis=1)
                           for j in range(4)], axis=1)
    wcat = np.concatenate([q_, k_, v_, ucat, gl, f(inputs["w_o_attn"])[0], f(inputs["w_pw2"])[0],
                           f(inputs["w_out"])[0]], axis=1)
    bc = lambda v, n=128: np.ascontiguousarray(np.broadcast_to(np.asarray(v, np.float32).reshape(1, -1), (n, np.size(v))))
    gvec = np.concatenate([bc(inputs["norm1_g"][0]), bc(inputs["norm2_g"][0]), bc(inputs["final_g"])], axis=1)
    L = NMETA + S
    half = 8
    inv_freq = 500000.0 ** (-np.arange(0, 16, 2, dtype=np.float32) / 16.0)
    ang = np.arange(L, dtype=np.float32)[:, None] * inv_freq[None, :].astype(np.float32)
    rope = np.concatenate([np.cos(ang), np.sin(ang)], axis=1).astype(np.float32)
    shared = {
        "meta": f(inputs["meta_tokens"]),
        "wcat": np.ascontiguousarray(wcat),
        "ident": np.eye(128, dtype=np.float32),
        "ustr": np.ascontiguousarray(np.triu(np.ones((128, 128), dtype=np.float32), k=1)),
        "gvec": np.ascontiguousarray(gvec),
        "rope": np.ascontiguousarray(rope),
        "convw": np.ascontiguousarray(f(inputs["conv_w"])[0].T),
        "cvec": np.ascontiguousarray(np.stack([f(inputs["conv_b"])[0], f(inputs["conv_ln_g"])[0],
                                               f(inputs["conv_ln_b"])[0]], axis=1)),
        "lamv": np.ascontiguousarray(np.concatenate([bc(inputs["lam_q1"][0]), bc(inputs["lam_k1"][0]),
                                                     bc(inputs["lam_q2"][0]), bc(inputs["lam_k2"][0])], axis=1)),
        "subg": bc(inputs["subln_g"][0]),
        "wr": np.ascontiguousarray(np.concatenate([f(inputs["w_group"])[0], f(inputs["w_router"])[0]], axis=1)),
        "br": np.ascontiguousarray(np.concatenate([bc(inputs["b_group"][0]), bc(inputs["b_router"][0])], axis=1)),
        "weg": np.ascontiguousarray(f(inputs["w_e_gate"])[0].reshape(NE, 8, 128, DE).transpose(0, 2, 1, 3)).reshape(NE * 256, 2048),
        "weu": np.ascontiguousarray(f(inputs["w_e_up"])[0].reshape(NE, 8, 128, DE).transpose(0, 2, 1, 3)).reshape(NE * 256, 2048),
        "wed": np.ascontiguousarray(f(inputs["w_e_down"])[0].reshape(NE, 4, 128, D).transpose(0, 2, 1, 3)).reshape(NE * 256, 2048),
    }
    return shared


def kernel(**inputs):
    x = np.asarray(inputs["x"], dtype=np.float32)
    B, S, _ = x.shape
    ncores = NCORES if B % NCORES == 0 else B
    NSEQ = B // ncores
    nc = build_nc(NSEQ, S)
    shared = _host_inputs(inputs, NSEQ, S)
    in_maps = []
    for c in range(ncores):
        m = dict(shared)
        m["x"] = np.ascontiguousarray(x[c * NSEQ:(c + 1) * NSEQ].reshape(NSEQ * S, D))
        in_maps.append(m)
    res = run_bass_kernel_spmd(nc, in_maps, core_ids=list(range(ncores)))
    out = np.stack([np.asarray(r["y"], dtype=np.float32).reshape(NSEQ, S, D) for r in res.results], axis=0)
    return out.reshape(B, S, D)
```

```python
import math
import numpy as np
import concourse.bass as bass
import concourse.mybir as mybir
from concourse.bass_utils import run_bass_kernel_spmd

F32 = mybir.dt.float32
BF16 = mybir.dt.bfloat16
I32 = mybir.dt.int32
AF = mybir.ActivationFunctionType
ALU = mybir.AluOpType
AX = mybir.AxisListType

D = 1024
NMETA = 16
NH = 8
HD = 64
CONVK = 31
NE = 32
DE = 512
EPS = 1e-6
LAM_INIT = 0.8 - 0.6 * math.exp(-0.3 * 0)
NCORES = 8
TB = 512
NSLAB = 20
RSQ_MAGIC = 1597463007.0


class Buf:
    __slots__ = ("ap", "w", "r", "ex", "const")

    def __init__(self, ap=None, ex=False, const=False):
        self.ap = ap
        self.w = None
        self.r = []
        self.ex = ex
        self.const = const


class Op:
    __slots__ = ("id", "eng", "key", "inc", "fn", "deps", "cost", "lat", "num", "tag", "t0")


class Prog:
    ENG = ("pe", "act", "dve", "pool", "sp")
    DCOST = {"pe": 0.7, "act": 0.35, "dve": 0.25, "pool": 1.5, "sp": 0.15}

    def __init__(self, nc):
        self.nc = nc
        self.ops = []
        self.lo = 0
        self.cnt = {}
        self.waited = {e: {} for e in self.ENG}
        self.semh = {}
        self.ctx = []
        self.last = {}
        self.nosched = False
        self.tag = (0, 0)
        self.role = "front"
        self.split = {}
        self.splitmap = {}

    def sem(self, key):
        if key not in self.semh:
            cm = self.nc.semaphore("s_" + key)
            self.semh[key] = cm.__enter__()
            self.ctx.append(cm)
            self.cnt[key] = 0
        return self.semh[key]

    def _remap(self, bs):
        if not self.split:
            return bs
        out = []
        for b in bs:
            if id(b) in self.split:
                k = (id(b), self.role)
                if k not in self.splitmap:
                    self.splitmap[k] = Buf(ex=b.ex, const=b.const)
                out.append(self.splitmap[k])
            else:
                out.append(b)
        return out

    def _record(self, eng, key, inc, fn, reads, writes, extra, cost, lat):
        self.sem(key)
        reads = self._remap(reads)
        writes = self._remap(writes)
        ops = self.ops
        d = set()
        for b in reads:
            if b.w is not None:
                d.add(b.w)
            if b.ex:
                d.update(i for i in b.r if ops[i].eng != eng)
        for b in writes:
            if b.w is not None:
                d.add(b.w)
            d.update(b.r)
        d.update(t for t in extra if t is not None)
        o = Op()
        o.id = len(ops)
        o.eng = eng
        o.key = key
        o.inc = inc
        o.fn = fn
        o.deps = d
        o.cost = self.DCOST[eng] if cost is None else cost
        o.lat = lat
        o.num = None
        o.tag = self.tag
        o.t0 = 0.0
        ops.append(o)
        for b in writes:
            b.w = o.id
            b.r = []
        for b in reads:
            if not b.const:
                b.r.append(o.id)
        self.last[key] = o.id
        return o.id

    def op(self, eng, fn, reads=(), writes=(), extra=(), cost=None):
        return self._record(eng, eng, 1, fn, reads, writes, extra, cost, 0.5)

    def dma(self, eng, semkey, fn, reads=(), writes=(), extra=(), cost=None, lat=3.0):
        return self._record(eng, semkey, 16, fn, reads, writes, extra, cost, lat)

    def _schedule(self, phase):
        import heapq
        lo = self.lo
        order = {e: [] for e in self.ENG}
        if self.nosched:
            for o in phase:
                order[o.eng].append(o)
            return order
        indeg = {}
        succ = {}
        ready = {}
        for o in phase:
            n = 0
            for dd in o.deps:
                if dd >= lo:
                    n += 1
                    succ.setdefault(dd, []).append(o)
            indeg[o.id] = n
            ready[o.id] = 0.0
        fut = {e: [] for e in self.ENG}
        av = {e: [] for e in self.ENG}
        free = {e: 0.0 for e in self.ENG}
        import os as _os2
        mode = _os2.environ.get("KPRIO", "bl")
        wbl = float(_os2.environ.get("KPRIOW", "1.0"))
        bl = {}
        for o in reversed(phase):
            m_ = 0.0
            for s_ in succ.get(o.id, ()):
                if bl[s_.id] > m_:
                    m_ = bl[s_.id]
            bl[o.id] = o.cost + o.lat + m_
        if mode == "bl":
            def pk(o):
                return (-(bl[o.id] - wbl * 0.0), o.id)
        else:
            def pk(o):
                return (o.id, o.id)
        for o in phase:
            if indeg[o.id] == 0:
                heapq.heappush(av[o.eng], (pk(o), o.id, o))
        left = len(phase)
        while left:
            best = None
            for e in self.ENG:
                f = fut[e]
                while f and f[0][0] <= free[e]:
                    _, oid, oo = heapq.heappop(f)
                    heapq.heappush(av[e], (pk(oo), oid, oo))
                if av[e]:
                    cand = (free[e], av[e][0][1], e, True)
                elif f:
                    cand = (f[0][0], f[0][1], e, False)
                else:
                    continue
                if best is None or cand < best:
                    best = cand
            t, _, e, isav = best
            if isav:
                _, _, o = heapq.heappop(av[e])
            else:
                _, _, o = heapq.heappop(fut[e])
            free[e] = t + o.cost
            o.t0 = t
            fin = t + o.cost + o.lat
            order[e].append(o)
            left -= 1
            for s_ in succ.get(o.id, ()):
                if fin > ready[s_.id]:
                    ready[s_.id] = fin
                indeg[s_.id] -= 1
                if indeg[s_.id] == 0:
                    heapq.heappush(fut[s_.eng], (ready[s_.id], s_.id, s_))
        self.est = max(free.values())
        return order

    def flush(self, final_waits=()):
        nc = self.nc
        semh = self.semh
        if final_waits:
            self._record("sp", "sp", 1, None, (), (), [t for t in final_waits if t is not None], 0.05, 0.0)
        phase = self.ops[self.lo:]
        order = self._schedule(phase)
        self.lo = len(self.ops)
        for e in self.ENG:
            for o in order[e]:
                self.cnt[o.key] += o.inc
                o.num = self.cnt[o.key]
        ops = self.ops
        plan = {}
        for e in self.ENG:
            lst = []
            wd = self.waited[e]
            for o in order[e]:
                best = {}
                for dd in o.deps:
                    p = ops[dd]
                    if p.num > best.get(p.key, 0):
                        best[p.key] = p.num
                wl = []
                for key, val in best.items():
                    if wd.get(key, 0) >= val:
                        continue
                    wd[key] = val
                    wl.append((key, val))
                lst.append((o, wl))
            plan[e] = lst

        def run(e, lst):
            for o, wl in lst:
                for key, val in wl:
                    e.wait_ge(semh[key], val)
                if o.fn is None:
                    continue
                ins = o.fn(e)
                ins.then_inc(semh[o.key], o.inc)

        with nc.Block() as block:
            @block.tensor
            def _(e):
                run(e, plan["pe"])

            @block.scalar
            def _(e):
                run(e, plan["act"])

            @block.vector
            def _(e):
                run(e, plan["dve"])

            @block.gpsimd
            def _(e):
                run(e, plan["pool"])

            @block.sync
            def _(e):
                run(e, plan["sp"])

    def close(self):
        for cm in reversed(self.ctx):
            cm.__exit__(None, None, None)


class _Stop(Exception):
    pass


def build_nc(NSEQ, S, stop_phase1=False, stop_at=None):
    def ckpt(n):
        P.tag = (P.tag[0], n)
        if stop_at is not None and n == stop_at:
            raise _Stop()
    NT = S // 128
    NB = S // TB
    NTOK = NSEQ * S
    GT = min(16, NT)
    NG = (NSEQ * NT) // GT
    KC = S + NMETA

    nc = bass.Bass("TRN2", target_bir_lowering=False)
    dt_in = lambda name, shape: nc.dram_tensor(name, list(shape), F32, kind="ExternalInput").ap()
    x_d = dt_in("x", (NTOK, D))
    meta_d = dt_in("meta", (NMETA, D))
    wcat_d = dt_in("wcat", (D, NSLAB * 512))
    ident_d = dt_in("ident", (128, 128))
    ustr_d = dt_in("ustr", (128, 128))
    gvec_d = dt_in("gvec", (128, 3 * D))
    rope_d = dt_in("rope", (NMETA + S, 16))
    convw_d = dt_in("convw", (D, CONVK))
    cvec_d = dt_in("cvec", (D, 3))
    lamv_d = dt_in("lamv", (128, 4 * HD))
    subg_d = dt_in("subg", (128, 128))
    wr_d = dt_in("wr", (D, 36))
    br_d = dt_in("br", (128, 36))
    weg_f = dt_in("weg", (NE * 256, 2048))
    weu_f = dt_in("weu", (NE * 256, 2048))
    wed_f = dt_in("wed", (NE * 256, 2048))
    y_d = nc.dram_tensor("y", [NTOK, D], F32, kind="ExternalOutput").ap()
    hres_d = y_d if stop_phase1 else nc.dram_tensor("hres", [NTOK, D], F32, kind="Internal").ap()

    P = Prog(nc)
    import os as _os
    if _os.environ.get('KNOSCHED'):
        P.nosched = True
    cms = []

    def sb(name, shape, dt):
        cm = nc.sbuf_tensor("sb_" + name, list(shape), dt)
        t = cm.__enter__()
        cms.append(cm)
        return t

    def free_to(n):
        while len(cms) > n:
            cms.pop().__exit__(None, None, None)

    pcm = [nc.psum_tensor("ps%d" % i, [128, 512], F32) for i in range(8)]
    ps = [c.__enter__() for c in pcm]
    psb = [Buf(t, ex=True) for t in ps]

    ident_f = sb("ident_f", (128, 128), F32)
    ident_b = sb("ident_b", (128, 128), BF16)
    gvec = sb("gvec", (128, D), F32)
    cosr = sb("cosr", (128, NT, 16), F32)
    ropem = sb("ropem", (16, 16), F32)
    convw = sb("convw", (128, 8, CONVK), F32)
    cvec = sb("cvec", (128, 8, 3), F32)
    hgb = sb("hgb", (128, 8, 2), F32)
    lamv = sb("lamv", (128, 4 * HD), F32)
    subg = sb("subg", (128, 128), F32)
    neglam = sb("neglam", (128, 1), F32)
    lamt = sb("lamt", (128, 4), F32)
    wr = sb("wr", (128, 8, 36), F32)
    br = sb("br", (128, 36), F32)
    ones_b = sb("ones_b", (128, 128), BF16)
    junkc = sb("junkc", (128, HD), BF16)
    B_const = Buf(const=True)
    B_junk = Buf()

    def ld(dst, src, key="c_ld"):
        return P.dma("sp", key, lambda e: e.dma_start(out=dst, in_=src), writes=[B_const])

    ld(ident_f[:], ident_d)
    ld(gvec[:], gvec_d[:, 0:D])
    ld(cosr[:], rope_d[NMETA:NMETA + S, :].rearrange("(t p) c -> p t c", p=128))
    ld(ropem[:], rope_d[0:NMETA, :])
    ld(convw[:], convw_d.rearrange("(c p) k -> p c k", p=128))
    ld(cvec[:], cvec_d.rearrange("(c p) k -> p c k", p=128))
    ld(lamv[:], lamv_d)
    ld(subg[:], subg_d)
    ld(wr[:], wr_d.rearrange("(c p) k -> p c k", p=128))
    ld(br[:], br_d)

    def dve(fn, reads=(), writes=(), extra=(), cost=None):
        return P.op("dve", fn, reads, writes, extra, cost)

    def act(fn, reads=(), writes=(), extra=(), cost=None):
        return P.op("act", fn, reads, writes, extra, cost)

    def pe(fn, reads=(), writes=(), extra=(), cost=None):
        return P.op("pe", fn, reads, writes, extra, cost)

    C = [B_const]
    dve(lambda e: e.tensor_copy(out=ident_b[:], in_=ident_f[:]), reads=C, writes=C)
    dve(lambda e: e.memset(ones_b[:], 1.0 / 1024.0), writes=C)
    dve(lambda e: e.tensor_scalar(out=convw[:], in0=convw[:], scalar1=0.5, scalar2=None, op0=ALU.mult),
        reads=C, writes=C)
    dve(lambda e: e.tensor_scalar(out=subg[:], in0=subg[:], scalar1=2.0 * (1.0 - LAM_INIT), scalar2=None,
                                  op0=ALU.mult), reads=C, writes=C)
    dve(lambda e: e.tensor_scalar(out=hgb[:], in0=cvec[:, :, 1:3], scalar1=0.5, scalar2=None, op0=ALU.mult),
        reads=C, writes=C)
    dve(lambda e: e.scalar_tensor_tensor(out=junkc[:, 0:HD], in0=lamv[:, 0:HD], scalar=1.0,
                                         in1=lamv[:, HD:2 * HD], op0=ALU.mult, op1=ALU.mult,
                                         accum_out=lamt[:, 0:1]), reads=C, writes=C)
    dve(lambda e: e.scalar_tensor_tensor(out=junkc[:, 0:HD], in0=lamv[:, 2 * HD:3 * HD], scalar=1.0,
                                         in1=lamv[:, 3 * HD:4 * HD], op0=ALU.mult, op1=ALU.mult,
                                         accum_out=lamt[:, 1:2]), reads=C, writes=C)
    act(lambda e: e.activation(out=lamt[:, 2:4], in_=lamt[:, 0:2], func=AF.Exp), reads=C, writes=C)
    dve(lambda e: e.tensor_tensor(out=neglam[:], in0=lamt[:, 3:4], in1=lamt[:, 2:3], op=ALU.subtract),
        reads=C, writes=C)
    dve(lambda e: e.tensor_scalar(out=neglam[:], in0=neglam[:], scalar1=-LAM_INIT, scalar2=None, op0=ALU.add),
        reads=C, writes=C)

    g1 = gvec[:, 0:D]

    rs_y = sb("rs_y", (128, 8), F32)
    rs_t = sb("rs_t", (128, 8), F32)
    B_rs = Buf()

    def rsqrt(vb, v_ap, out_b, out_ap, pn, shape2, scr=None):
        n = shape2
        if scr is None:
            y = rs_y[:pn, 0:n]
            t = rs_t[:pn, 0:n]
            R = [B_rs]
        else:
            y, t, R = scr
        rc = 0.12 + n / 960.0
        dve(lambda e: e.tensor_scalar(out=t.bitcast(I32), in0=v_ap.bitcast(I32), scalar1=1, scalar2=None,
                                      op0=ALU.arith_shift_right), reads=[vb], writes=R, cost=rc)
        dve(lambda e: e.tensor_scalar(out=y.bitcast(I32), in0=t.bitcast(I32), scalar1=-1.0, scalar2=RSQ_MAGIC,
                                      op0=ALU.mult, op1=ALU.add), reads=R, writes=R, cost=rc)
        for it in range(3):
            dve(lambda e: e.tensor_tensor(out=t, in0=y, in1=y, op=ALU.mult), reads=R, writes=R, cost=rc)
            dve(lambda e: e.tensor_tensor(out=t, in0=t, in1=v_ap, op=ALU.mult), reads=R + [vb], writes=R, cost=rc)
            dve(lambda e: e.tensor_scalar(out=t, in0=t, scalar1=-0.5, scalar2=1.5, op0=ALU.mult, op1=ALU.add),
                reads=R, writes=R, cost=rc)
            if it < 2:
                dve(lambda e: e.tensor_tensor(out=y, in0=y, in1=t, op=ALU.mult), reads=R, writes=R, cost=rc)
            else:
                dve(lambda e: e.tensor_tensor(out=out_ap, in0=y, in1=t, op=ALU.mult), reads=R, writes=[out_b], cost=rc)

    n_const = len(cms)
    try:
        ckpt(0)
    except _Stop:
        P.flush(); free_to(0)
        for c in reversed(pcm):
            c.__exit__(None, None, None)
        P.close()
        return nc

    KT = sb("KT", (128, NH, KC), BF16)
    VA = sb("VA", (128, NT, NH, 130), BF16)
    VM = sb("VM", (16, NH, 130), BF16)
    NRING = int(_os.environ.get("KRING", "2"))
    NXT = int(_os.environ.get("KXT", "1"))
    ring = [sb("ring%d" % i, (128, 8, 512), BF16) for i in range(NRING)]
    hT = sb("hT", (128, 8, TB), BF16)
    QT = sb("QT", (128, NH, 2, TB), BF16)
    z2Ts = [sb("z2T%d" % i, (128, 8, 30 + TB), BF16) for i in range(2)]
    cacc = [sb("cacc%d" % i, (128, TB), F32) for i in range(2)]
    z2m = sb("z2m", (128, 8, 16), BF16)
    zc = sb("zc", (128, 8, TB), BF16)
    onT = sb("onT", (128, NH, TB), BF16)
    sT = onT
    mixT = sb("mixT", (128, 8, TB), BF16)
    thg = sb("thg", (128, 16, TB), BF16)
    xt = [sb("xt%d" % i, (128, D), F32) for i in range(NXT)]
    hb = sb("hb", (128, D), BF16)
    qkt = [sb("qkt0", (128, 512), BF16)]
    rtmp = sb("rtmp", (128, 4, 64), F32)
    pTf = [sb("pTf%d" % i, (128, 512), BF16) for i in range(3)]
    pTd = [sb("pTd%d" % i, (128, 512), BF16) for i in range(2)]
    o_all = sb("o_all", (128, NH, 128), F32)
    on_all = sb("on_all", (128, NH * 128), BF16)
    sm = sb("sm", (128, 32), F32)
    smA = sb("smA", (128, 2, 4), F32)
    smH = sb("smH", (128, NH, 4), F32)
    rsA = sb("rsA", (128, 2, 2), F32)
    rsQ = sb("rsQ", (128, 2, 8), F32)
    B_smA = [Buf(), Buf()]; B_rsA = [Buf(), Buf()]; B_smH = [Buf() for _ in range(NH)]; B_ssH = [Buf() for _ in range(NH)]
    B_rsQ = Buf(); B_oallh = [Buf() for _ in range(NH)]; B_onallh = [Buf() for _ in range(NH)]
    lnm = sb("lnm", (128, TB), F32)
    lnr = sb("lnr", (128, TB), F32)
    ctmp = [sb("ctmp%d" % i, (128, TB), F32) for i in range(2)]
    cbf = [sb("cbf%d" % i, (128, TB), BF16) for i in range(2)]

    B_KT = [Buf() for _ in range(NB * NSEQ + 1)]
    B_VA = [Buf() for _ in range(NB * NSEQ + 1)]
    B_ring = [Buf() for _ in range(NRING)]
    B_hT = Buf(); B_QT = Buf(); B_z2Ts = [Buf(), Buf()]; B_z2m = Buf(); B_cacc = [Buf(), Buf()]
    B_onT = Buf(); B_thg = Buf(); B_mixT = Buf(); B_sT = B_onT
    B_xt = [Buf(), Buf()]; B_hb = Buf(); B_qkt = [Buf(), Buf()]; B_rtmp = Buf()
    B_pTf = [Buf() for _ in range(3)]
    B_pTd = [Buf(), Buf()]
    B_oall = Buf(); B_onall = Buf(); B_sm = Buf(); B_lnm = Buf(); B_lnr = Buf()
    B_ctmp = [Buf(), Buf()]; B_cbf = [Buf(), Buf()]
    B_meta = Buf()

    dve(lambda e: e.memset(VA[:, :, :, 128:130], 1.0), writes=B_VA)
    dve(lambda e: e.memset(QT[:], 0.0), writes=[B_QT], cost=4.0)
    dve(lambda e: e.memset(VM[:, :, 128:130], 1.0), writes=[B_meta])

    state = {"g": 0, "slab": 0, "xs": 0}

    NGB = int(_os.environ.get("KGB", "6"))

    SPLIT = _os.environ.get("KSPLIT", "0") == "1"

    POOLS = _os.environ.get("KPOOLS", "1") == "1"

    def gbank():
        if POOLS:
            if state.get("role", "front") == "front":
                b = state["g"] % 2
                state["g"] += 1
                return b
            b = 2 + (state.setdefault("sp", 0) % 4)
            state["sp"] += 1
            return b
        if SPLIT:
            return 0 if state.get("role", "front") == "front" else 1
        n = 2 if state.get("attn") else NGB
        b = state["g"] % n
        state["g"] += 1
        return b

    def load_slab2(si):
        slot = state["slab"] % NRING
        state["slab"] += 1
        src = wcat_d[:, si * 512:(si + 1) * 512].rearrange("(c p) n -> p c n", p=128)
        b = B_ring[slot]
        t0 = P.dma("pool", "ring%d_0" % slot,
                   lambda e: e.dma_start(out=ring[slot][:, 0:4, :], in_=src[:, 0:4, :]), writes=[b], cost=1.0, lat=8.0)
        b2 = B_ring2[slot]
        t1 = P.dma("pool", "ring%d_1" % slot,
                   lambda e: e.dma_start(out=ring[slot][:, 4:8, :], in_=src[:, 4:8, :]), writes=[b2], cost=1.0, lat=8.0)
        return slot

    B_ring2 = [Buf() for _ in range(NRING)]

    def ring_bufs(slot):
        return [B_ring[slot], B_ring2[slot]]

    def stage_norm(src_ap, n, off):
        xi = state["xs"] % 2
        state["xs"] += 1
        xb = B_xt[xi % NXT]
        xtile = xt[xi % NXT]
        P.dma("sp", "xt%d" % (xi % NXT), lambda e: e.dma_start(out=xtile[:n, :], in_=src_ap), writes=[xb])
        sa = smA[:, xi, :]
        sab = B_smA[xi]
        act(lambda e: e.activation(out=hb[:n, :], in_=xtile[:n, :], func=AF.Square, accum_out=sa[:n, 0:1]),
            reads=[xb], writes=[B_hb, sab], cost=0.95)
        dve(lambda e: e.tensor_scalar(out=sa[:n, 1:2], in0=sa[:n, 0:1], scalar1=1.0 / D, scalar2=EPS,
                                      op0=ALU.mult, op1=ALU.add), reads=[sab], writes=[sab])
        rsqrt(sab, sa[:n, 1:2], sab, sa[:n, 2:3], n, 1, scr=(rsA[:n, xi, 0:1], rsA[:n, xi, 1:2], [B_rsA[xi]]))
        dve(lambda e: e.scalar_tensor_tensor(out=hb[:n, :], in0=xtile[:n, :], scalar=sa[:n, 2:3], in1=g1[:n, :],
                                             op0=ALU.mult, op1=ALU.mult), reads=[xb, sab] + C, writes=[B_hb], cost=1.2)
        bk = gbank()
        pbf = ps[bk][:].bitcast(BF16)

        def tr(e):
            ins = None
            for c in range(8):
                ins = e.transpose(out=pbf[:, c * 128:c * 128 + n], in_=hb[:n, c * 128:(c + 1) * 128],
                                  identity=ident_b[:n, :n])
            return ins
        pe(tr, reads=[B_hb] + C, writes=[psb[bk]], cost=0.5)
        act(lambda e: e.activation(out=hT[:, :, off:off + n],
                                   in_=pbf.rearrange("p (c t) -> p c t", c=8)[:, :, 0:n], func=AF.Copy),
            reads=[psb[bk]], writes=[B_hT], cost=0.9)

    def proj_tm(slot, off, n):
        bk = gbank()

        def mm(e):
            ins = None
            for kc in range(8):
                ins = e.matmul(out=ps[bk][:n, :], lhsT=hT[:, kc, off:off + n], rhs=ring[slot][:, kc, :],
                               start=(kc == 0), stop=(kc == 7))
            return ins
        pe(mm, reads=[B_hT] + ring_bufs(slot), writes=[psb[bk]], cost=2.3)
        return bk

    def rope_evac(bk, n, cs_ap, dst_write, dst_bufs):
        qi = 0
        state["qk"] = state.get("qk", 0) + 1
        qb = B_qkt[qi]
        qk = qkt[qi]
        pv = ps[bk][:n, :].rearrange("p (g d) -> p g d", g=8)
        qv = qk[:n, :].rearrange("p (g d) -> p g d", g=8)
        cosb = cs_ap[:, 0:8].unsqueeze(1).to_broadcast([n, 8, 8])
        sinb = cs_ap[:, 8:16].unsqueeze(1).to_broadcast([n, 8, 8])
        act(lambda e: e.activation(out=qv[:, :, 16:64], in_=pv[:, :, 16:64], func=AF.Copy),
            reads=[psb[bk]], writes=[qb])
        t = [rtmp[:n, j, :].rearrange("p (g d) -> p g d", g=8) for j in range(4)]
        rd = [psb[bk]] + C
        dve(lambda e: e.tensor_tensor(out=t[0], in0=pv[:, :, 0:8], in1=cosb, op=ALU.mult), reads=rd, writes=[B_rtmp])
        dve(lambda e: e.tensor_tensor(out=t[1], in0=pv[:, :, 8:16], in1=sinb, op=ALU.mult), reads=rd, writes=[B_rtmp])
        dve(lambda e: e.tensor_tensor(out=t[2], in0=pv[:, :, 8:16], in1=cosb, op=ALU.mult), reads=rd, writes=[B_rtmp])
        dve(lambda e: e.tensor_tensor(out=t[3], in0=pv[:, :, 0:8], in1=sinb, op=ALU.mult), reads=rd, writes=[B_rtmp])
        dve(lambda e: e.tensor_tensor(out=qv[:, :, 0:8], in0=t[0], in1=t[1], op=ALU.subtract),
            reads=[B_rtmp], writes=[qb])
        dve(lambda e: e.tensor_tensor(out=qv[:, :, 8:16], in0=t[2], in1=t[3], op=ALU.add),
            reads=[B_rtmp], writes=[qb])
        b2 = gbank()
        pbf = ps[b2][:].bitcast(BF16)

        def tr(e):
            ins = None
            for hh in range(4):
                ins = e.transpose(out=pbf[:, hh * 128:hh * 128 + n], in_=qk[:n, hh * 128:(hh + 1) * 128],
                                  identity=ident_b[:n, :n])
            return ins
        pe(tr, reads=[qb] + C, writes=[psb[b2]])
        act(lambda e: dst_write(e, pbf[:, 0:512].rearrange("p (h t) -> p h t", h=4)[:, :, 0:n]),
            reads=[psb[b2]], writes=dst_bufs)

    def proj_fm(slot, cc, ntok):
        bk = gbank()

        def mm(e):
            ins = None
            for kc in range(8):
                ins = e.matmul(out=ps[bk][:, 0:ntok], lhsT=ring[slot][:, kc, cc * 128:(cc + 1) * 128],
                               rhs=hT[:, kc, 0:ntok], start=(kc == 0), stop=(kc == 7))
            return ins
        pe(mm, reads=[B_hT] + ring_bufs(slot), writes=[psb[bk]], cost=8 * (0.04 + ntok / 1950.0))
        return bk

    def glu_pair(slot, q, ntok, dst_ap, dst_b):
        ba = proj_fm(slot, q, ntok)
        if SPLIT:
            act(lambda e: e.activation(out=ctmp[0][:, 0:ntok], in_=ps[ba][:, 0:ntok], func=AF.Copy),
                reads=[psb[ba]], writes=[B_ctmp[0]], cost=0.22 + ntok / 1400.0)
        bb = proj_fm(slot, 2 + q, ntok)
        act(lambda e: e.activation(out=ctmp[1][:, 0:ntok], in_=ps[bb][:, 0:ntok], func=AF.Tanh, scale=0.5),
            reads=[psb[bb]], writes=[B_ctmp[1]], cost=0.22 + ntok / 1400.0)
        if SPLIT:
            dve(lambda e: e.scalar_tensor_tensor(out=dst_ap, in0=ctmp[1][:, 0:ntok], scalar=1.0,
                                                 in1=ctmp[0][:, 0:ntok], op0=ALU.add, op1=ALU.mult),
                reads=[B_ctmp[0], B_ctmp[1]], writes=[dst_b], cost=0.1 + ntok / 960.0)
        else:
            dve(lambda e: e.scalar_tensor_tensor(out=dst_ap, in0=ctmp[1][:, 0:ntok], scalar=1.0,
                                                 in1=ps[ba][:, 0:ntok], op0=ALU.add, op1=ALU.mult),
                reads=[B_ctmp[1], psb[ba]], writes=[dst_b], cost=0.1 + ntok / 960.0)

    def meta_pass():
        ckpt(1)
        stage_norm(meta_d, NMETA, 0)
        ckpt(2)
        for si in (2, 3):
            slot = load_slab2(si)
            bk = proj_tm(slot, 0, NMETA)
            h0 = (si - 2) * 4
            rope_evac(bk, NMETA, ropem[:, :],
                      lambda e, src, h0=h0: e.activation(out=KT[:, h0:h0 + 4, 0:NMETA], in_=src, func=AF.Copy),
                      [B_meta])
        ckpt(3)
        for si in (4, 5):
            slot = load_slab2(si)
            bk = proj_tm(slot, 0, NMETA)
            h0 = (si - 4) * 4
            act(lambda e, bk=bk, h0=h0: e.activation(out=VM[:, h0:h0 + 4, 0:128],
                                                     in_=ps[bk][:NMETA, :].rearrange("p (h d) -> p h d", h=4),
                                                     func=AF.Copy), reads=[psb[bk]], writes=[B_meta])
        ckpt(4)
        for j in range(4):
            slot = load_slab2(6 + j)
            for q in range(2):
                glu_pair(slot, q, NMETA, z2m[:, 2 * j + q, :], B_z2m)
        ckpt(5)

    def attention_qtile(seq, blk, qt, conv_work):
        gi = blk * 4 + qt
        q0 = qt * 128
        kvb = [B_KT[b] for b in range(blk + 1)] + [B_meta]
        vvb = [B_VA[b] for b in range(blk + 1)] + [B_meta]
        for h in range(NH):
            ob = 6 + (state.setdefault("ob", 0) % 2)
            state["ob"] += 1
            units = [("d", gi)]
            j = 0
            while j < gi:
                units.append(("f", j, min(j + 2, gi)))
                j += 2
            first = [True]
            nun = len(units)

            def emit_qk(ui, h=h):
                u = units[ui]
                bank = 2 + (state.setdefault("sp", 0) % 4)
                state["sp"] += 1
                if u[0] == "d":
                    kts = [(16 + gi * 128, 128, 0), (0, 16, 256)]
                else:
                    kts = [(16 + jj * 128, 128, (jj - u[1]) * 256) for jj in range(u[1], u[2])]

                def qk(e, kts=kts, bank=bank, h=h):
                    ins = None
                    for (kc0, nk, co) in kts:
                        ins = e.matmul(out=ps[bank][:nk, co:co + 256].rearrange("p (m q) -> p m q", m=2),
                                       lhsT=KT[:, h, kc0:kc0 + nk], rhs=QT[:, h, :, q0:q0 + 128], start=True, stop=True)
                    return ins
                pe(qk, reads=kvb + [B_QT], writes=[psb[bank]], cost=0.15 * len(kts))
                return bank, kts

            info = emit_qk(0)
            for ui, u in enumerate(units):
                nxt = emit_qk(ui + 1) if ui + 1 < nun else None
                bank, kts = info
                info = nxt
                width = 256 * len(kts)
                S_ = ps[bank]
                if u[0] == "d":
                    pi = state.setdefault("pd", 0) % 2
                    state["pd"] += 1
                    pt = pTd[pi]
                    pb = B_pTd[pi]
                    act(lambda e, pt=pt, S_=S_: e.activation(out=pt[:, 0:256], in_=S_[:, 0:256], func=AF.Exp, scale=0.125),
                        reads=[psb[bank]], writes=[pb], cost=0.4)
                    act(lambda e, pt=pt, S_=S_: e.activation(out=pt[0:16, 256:512], in_=S_[0:16, 256:512], func=AF.Exp,
                                                             scale=0.125), reads=[psb[bank]], writes=[pb], cost=0.4)
                else:
                    pi = state.setdefault("pf", 0) % 3
                    state["pf"] += 1
                    pt = pTf[pi]
                    pb = B_pTf[pi]
                    act(lambda e, pt=pt, S_=S_, width=width: e.activation(out=pt[:, 0:width], in_=S_[:, 0:width],
                                                                         func=AF.Exp, scale=0.125),
                        reads=[psb[bank]], writes=[pb], cost=0.22 + width / 1400.0)
                specs = []
                for m in range(2):
                    for idx, (kc0, nk, co) in enumerate(kts):
                        c0 = co + m * 128
                        last = (ui == nun - 1 and m == 1 and idx == len(kts) - 1)
                        if u[0] == "d" and idx == 0:
                            specs.append((0, 64, 0, 128, c0, VA[0:64, gi, h, 0:129], first[0], False, m))
                            first[0] = False
                            specs.append((64, 128, 64, 128, c0 + 64, VA[64:128, gi, h, 0:129], False, last, m))
                        elif u[0] == "d":
                            specs.append((0, 16, 0, 128, c0, VM[0:16, h, 0:129], first[0], last, m))
                            first[0] = False
                        else:
                            specs.append((0, 128, 0, 128, c0, VA[:, (kc0 - 16) // 128, h, 0:129], first[0], last, m))
                            first[0] = False

                def pv(e, pt=pt, ob=ob, specs=specs):
                    ins = None
                    for (k0, k1, q_0, q_1, c0, rhs, st, sp_, m) in specs:
                        ins = e.matmul(out=ps[ob][q_0:q_1, m * 256:m * 256 + 129], lhsT=pt[k0:k1, c0:c0 + (q_1 - q_0)],
                                       rhs=rhs, start=st, stop=sp_, skip_group_check=True)
                    return ins
                pe(pv, reads=[pb] + vvb, writes=[psb[ob]], cost=0.125 * len(specs))
            O = ps[ob]
            sh = smH[:, h, :]
            shb = B_smH[h]
            dve(lambda e, O=O, sh=sh: e.reciprocal(out=sh[:, 0:1], in_=O[:, 128:129]), reads=[psb[ob]], writes=[shb])
            dve(lambda e, O=O, sh=sh: e.reciprocal(out=sh[:, 1:2], in_=O[:, 384:385]), reads=[psb[ob]], writes=[shb])
            dve(lambda e, sh=sh: e.tensor_tensor(out=sh[:, 2:3], in0=sh[:, 1:2], in1=neglam[:], op=ALU.mult),
                reads=[shb] + C, writes=[shb])
            dve(lambda e, O=O, h=h, sh=sh: e.tensor_scalar(out=o_all[:, h, :], in0=O[:, 0:128], scalar1=sh[:, 0:1],
                                                           scalar2=None, op0=ALU.mult),
                reads=[psb[ob], shb], writes=[B_oallh[h]])
            dve(lambda e, O=O, h=h, sh=sh: e.scalar_tensor_tensor(out=o_all[:, h, :], in0=O[:, 256:384], scalar=sh[:, 2:3],
                                                                  in1=o_all[:, h, :], op0=ALU.mult, op1=ALU.add),
                reads=[psb[ob], shb], writes=[B_oallh[h]])
            dve(lambda e, h=h: e.scalar_tensor_tensor(out=on_all[:, h * 128:(h + 1) * 128], in0=o_all[:, h, :], scalar=1.0,
                                                      in1=o_all[:, h, :], op0=ALU.mult, op1=ALU.mult,
                                                      accum_out=sm[:, 8 + h:9 + h]),
                reads=[B_oallh[h]], writes=[B_onallh[h], B_ssH[h]])
            if conv_work:
                conv_work.pop(0)()
        qp = state.setdefault("qp", 0) % 2
        state["qp"] += 1
        dve(lambda e: e.tensor_scalar(out=sm[:, 16:24], in0=sm[:, 8:16], scalar1=1.0 / 128.0, scalar2=EPS,
                                      op0=ALU.mult, op1=ALU.add), reads=B_ssH, writes=[B_sm])
        rsqrt(B_sm, sm[:, 16:24], B_sm, sm[:, 24:32], 128, 8, scr=(rsQ[:, 0, :], rsQ[:, 1, :], [B_rsQ]))
        dve(lambda e: e.tensor_tensor(out=o_all[:], in0=o_all[:],
                                      in1=sm[:, 24:32].unsqueeze(2).to_broadcast([128, 8, 128]), op=ALU.mult),
            reads=[B_sm] + B_oallh, writes=B_oallh, cost=1.15)
        dve(lambda e: e.tensor_tensor(out=on_all[:].rearrange("p (h d) -> p h d", h=8), in0=o_all[:],
                                      in1=subg[:].unsqueeze(1).to_broadcast([128, 8, 128]), op=ALU.mult),
            reads=B_oallh + C, writes=B_onallh, cost=1.15)
        bk = gbank()
        pbf = ps[bk][:].bitcast(BF16)

        def tr(e):
            ins = None
            for h in range(8):
                ins = e.transpose(out=pbf[:, h * 128:(h + 1) * 128], in_=on_all[:, h * 128:(h + 1) * 128],
                                  identity=ident_b[:])
            return ins
        pe(tr, reads=B_onallh + C, writes=[psb[bk]], cost=0.5)
        act(lambda e: e.activation(out=onT[:, :, q0:q0 + 128], in_=pbf.rearrange("p (h t) -> p h t", h=8),
                                   func=AF.Copy), reads=[psb[bk]], writes=[B_onT], cost=0.95)

    B_S = [[Buf(), Buf()], [Buf(), Buf()]]

    def conv_chunk(cc, z2T, B_z2T):
        def w():
            k = state.setdefault("ca", 0) % 2
            state["ca"] += 1
            acc = cacc[k]
            ab = B_cacc[k]
            dve(lambda e: e.tensor_scalar(out=acc[:], in0=z2T[:, cc, 0:TB], scalar1=convw[:, cc, 0:1],
                                          scalar2=cvec[:, cc, 0:1], op0=ALU.mult, op1=ALU.add),
                reads=[B_z2T] + C, writes=[ab], cost=0.63)
            for j in range(1, CONVK - 1):
                dve(lambda e, j=j: e.scalar_tensor_tensor(out=acc[:], in0=z2T[:, cc, j:j + TB],
                                                          scalar=convw[:, cc, j:j + 1], in1=acc[:],
                                                          op0=ALU.mult, op1=ALU.add),
                    reads=[B_z2T] + C, writes=[ab], cost=0.63)
            j = CONVK - 1
            dve(lambda e: e.scalar_tensor_tensor(out=zc[:, cc, :], in0=z2T[:, cc, j:j + TB],
                                                 scalar=convw[:, cc, j:j + 1], in1=acc[:],
                                                 op0=ALU.mult, op1=ALU.add),
                reads=[B_z2T, ab] + C, writes=[B_zcp[cc]], cost=0.63)
        return w

    B_zcp = [Buf() for _ in range(8)]

    def block(seq, blk, part):
        tok0 = seq * S + blk * TB
        nonlocal B_thg, B_QT, B_hT, B_onT, B_sT, B_mixT, B_zcp
        if _os.environ.get("KFAKE"):
            par = (seq * NB + blk) % 2
            fk = state.setdefault("fk", {})
            if par not in fk:
                fk[par] = dict(thg=Buf(), QT=Buf(), hT=Buf(), onT=Buf(), mixT=Buf(), zcp=[Buf() for _ in range(8)])
            f_ = _os.environ["KFAKE"]
            if "t" in f_: B_thg = fk[par]["thg"]
            if "q" in f_: B_QT = fk[par]["QT"]
            if "h" in f_: B_hT = fk[par]["hT"]
            if "o" in f_: B_onT = fk[par]["onT"]; B_sT = B_onT
            if "m" in f_: B_mixT = fk[par]["mixT"]
            if "z" in f_: B_zcp = fk[par]["zcp"]
        P.tag = (seq * NB + blk + 1, 5)
        state["role"] = "front" if part in ("f1", "f2") else "back"
        P.role = state["role"]
        state["attn"] = (part == "attn")
        zi = (seq * NB + blk) % 2
        z2T = z2Ts[zi]
        B_z2T = B_z2Ts[zi]
        z2Tp = z2Ts[1 - zi]
        B_z2Tp = B_z2Ts[1 - zi]
        kb = B_KT[blk]
        vb = B_VA[blk]
        if part == "f1":
            for t in range(4):
                stage_norm(x_d[tok0 + t * 128:tok0 + (t + 1) * 128, :], 128, t * 128)
            ckpt(6)
        def stage_b(sis):
            for si in sis:
                slot = load_slab2(si)
                for t in range(4):
                    bk = proj_tm(slot, t * 128, 128)
                    gt = blk * 4 + t
                    if si < 2:
                        h0 = si * 4
                        def qdst(e, src, h0=h0, t=t):
                            e.activation(out=QT[0:64, h0:h0 + 4, 0, t * 128:(t + 1) * 128], in_=src[0:64], func=AF.Copy)
                            return e.activation(out=QT[64:128, h0:h0 + 4, 1, t * 128:(t + 1) * 128], in_=src[64:128],
                                                func=AF.Copy)
                        rope_evac(bk, 128, cosr[:, gt, :], qdst, [B_QT])
                    elif si < 4:
                        h0 = (si - 2) * 4
                        c0 = NMETA + gt * 128
                        rope_evac(bk, 128, cosr[:, gt, :],
                                  lambda e, src, h0=h0, c0=c0: e.activation(out=KT[:, h0:h0 + 4, c0:c0 + 128],
                                                                            in_=src, func=AF.Copy), [kb])
                    else:
                        h0 = (si - 4) * 4
                        act(lambda e, bk=bk, h0=h0, gt=gt: e.activation(
                            out=VA[:, gt, h0:h0 + 4, 0:128], in_=ps[bk][:, :].rearrange("p (h d) -> p h d", h=4),
                            func=AF.Copy), reads=[psb[bk]], writes=[vb])

        if part == "f1":
            stage_b((2, 3, 4, 5))
            ckpt(7)
            if blk == 0:
                dve(lambda e: e.memset(z2T[:, :, 0:14], 0.0), writes=[B_z2T])
                dve(lambda e: e.tensor_copy(out=z2T[:, :, 14:30], in_=z2m[:]), reads=[B_z2m], writes=[B_z2T])
            else:
                dve(lambda e: e.tensor_copy(out=z2T[:, :, 0:30], in_=z2Tp[:, :, TB:TB + 30]), reads=[B_z2Tp], writes=[B_z2T])
            for j in range(4):
                slot = load_slab2(6 + j)
                for q in range(2):
                    glu_pair(slot, q, TB, z2T[:, 2 * j + q, 30:30 + TB], B_z2T)
        if part == "f2":
            P.tag = (P.tag[0], 6)
            stage_b((0, 1))
            ckpt(9)
            for j in range(4):
                slot = load_slab2(10 + j)
                for cc in range(4):
                    bk = proj_fm(slot, cc, TB)
                    act(lambda e, bk=bk, idx=j * 4 + cc: e.activation(out=thg[:, idx, :], in_=ps[bk][:, :], func=AF.Tanh,
                                                                      scale=0.5), reads=[psb[bk]], writes=[B_thg], cost=0.6)
        if part == "attn":
            ckpt(8)
            conv_work = [conv_chunk(cc, z2T, B_z2T) for cc in range(8)]
            state["attn"] = True
            state["role"] = "back"
            P.role = "back"
            for qt in range(4):
                cw = []
                for h in range(NH):
                    cw.append(conv_work.pop(0) if (h % 4 == 1 and conv_work) else (lambda: None))
                attention_qtile(seq, blk, qt, cw)
            state["attn"] = False
            while conv_work:
                conv_work.pop(0)()
        if part == "back":
            ckpt(10)
            for j in range(2):
                slot = load_slab2(14 + j)
                for cc in range(4):
                    dc = j * 4 + cc
                    bk = gbank()

                    def mm(e, slot=slot, cc=cc, bk=bk):
                        ins = None
                        for h in range(8):
                            ins = e.matmul(out=ps[bk][:, :], lhsT=ring[slot][:, h, cc * 128:(cc + 1) * 128],
                                           rhs=onT[:, h, :], start=(h == 0), stop=(h == 7))
                        return ins
                    pe(mm, reads=[B_onT] + ring_bufs(slot), writes=[psb[bk]], cost=2.3)
                    dve(lambda e, dc=dc, bk=bk: e.scalar_tensor_tensor(out=mixT[:, dc, :], in0=thg[:, dc, :], scalar=1.0,
                                                                      in1=ps[bk][:, :], op0=ALU.add, op1=ALU.mult),
                        reads=[B_thg, psb[bk]], writes=[B_mixT], cost=0.63)
            ckpt(11)
            b_mean = gbank()
            for cc in range(8):
                pe(lambda e, cc=cc: e.matmul(out=ps[b_mean][:, :], lhsT=ones_b[:], rhs=zc[:, cc, :],
                                             start=(cc == 0), stop=(cc == 7)),
                   reads=[B_zcp[cc]] + C, writes=[psb[b_mean]], cost=0.25)
            dve(lambda e: e.tensor_copy(out=lnm[:], in_=ps[b_mean][:, :]), reads=[psb[b_mean]], writes=[B_lnm], cost=0.63)
            b_msq = gbank()
            state.setdefault("cb", 0)
            for cc in range(8):
                ci2 = state["cb"] % 2
                state["cb"] += 1
                act(lambda e, cc=cc, ci2=ci2: e.activation(out=cbf[ci2][:], in_=zc[:, cc, :], func=AF.Square),
                    reads=[B_zcp[cc]], writes=[B_cbf[ci2]], cost=0.6)
                pe(lambda e, cc=cc, ci2=ci2: e.matmul(out=ps[b_msq][:, :], lhsT=ones_b[:], rhs=cbf[ci2][:],
                                                      start=(cc == 0), stop=(cc == 7)),
                   reads=[B_cbf[ci2]] + C, writes=[psb[b_msq]], cost=0.25)
            dve(lambda e: e.tensor_tensor(out=lnr[:], in0=lnm[:], in1=lnm[:], op=ALU.mult), reads=[B_lnm], writes=[B_lnr])
            dve(lambda e: e.tensor_tensor(out=lnr[:], in0=ps[b_msq][:, :], in1=lnr[:], op=ALU.subtract),
                reads=[psb[b_msq], B_lnr], writes=[B_lnr])
            dve(lambda e: e.tensor_scalar(out=lnr[:], in0=lnr[:], scalar1=EPS, scalar2=None, op0=ALU.add),
                reads=[B_lnr], writes=[B_lnr])
            rsqrt(B_lnr, lnr[:], B_lnr, lnr[:], 128, TB, scr=(ctmp[0][:], ctmp[1][:], [B_ctmp[0], B_ctmp[1]]))
            for cc in range(8):
                ci = state.setdefault("ct", 0) % 2
                state["ct"] += 1
                cj = 1 - ci
                dve(lambda e, cc=cc, ci=ci: e.tensor_tensor(out=ctmp[ci][:], in0=zc[:, cc, :], in1=lnm[:], op=ALU.subtract),
                    reads=[B_zcp[cc], B_lnm], writes=[B_ctmp[ci]], cost=0.63)
                dve(lambda e, ci=ci: e.tensor_tensor(out=ctmp[ci][:], in0=ctmp[ci][:], in1=lnr[:], op=ALU.mult),
                    reads=[B_lnr], writes=[B_ctmp[ci]], cost=0.63)
                act(lambda e, cc=cc, ci=ci: e.activation(out=cbf[ci][:], in_=ctmp[ci][:], func=AF.Tanh,
                                                         scale=hgb[:, cc, 0:1], bias=hgb[:, cc, 1:2]),
                    reads=[B_ctmp[ci]] + C, writes=[B_cbf[ci]], cost=0.6)
                dve(lambda e, cc=cc, ci=ci: e.tensor_scalar(out=ctmp[ci][:], in0=ctmp[ci][:], scalar1=cvec[:, cc, 1:2],
                                                            scalar2=cvec[:, cc, 2:3], op0=ALU.mult, op1=ALU.add),
                    reads=C, writes=[B_ctmp[ci]], extra=[B_cbf[ci].w], cost=0.63)
                dve(lambda e, cc=cc, ci=ci: e.scalar_tensor_tensor(out=sT[:, cc, :], in0=cbf[ci][:], scalar=1.0,
                                                                  in1=ctmp[ci][:], op0=ALU.add, op1=ALU.mult),
                    reads=[B_cbf[ci], B_ctmp[ci]], writes=[B_sT], cost=0.63)
            ckpt(12)
            for j in range(2):
                slot = load_slab2(16 + j)
                for cc in range(4):
                    dc = j * 4 + cc
                    bk = gbank()

                    def mm(e, slot=slot, cc=cc, bk=bk):
                        ins = None
                        for c in range(8):
                            ins = e.matmul(out=ps[bk][:, :], lhsT=ring[slot][:, c, cc * 128:(cc + 1) * 128],
                                           rhs=sT[:, c, :], start=(c == 0), stop=(c == 7))
                        return ins
                    pe(mm, reads=[B_sT] + ring_bufs(slot), writes=[psb[bk]], cost=2.3)
                    ci = state["ct"] % 2
                    state["ct"] += 1
                    dve(lambda e, dc=dc, bk=bk, ci=ci: e.scalar_tensor_tensor(
                        out=ctmp[ci][:], in0=thg[:, 8 + dc, :], scalar=1.0, in1=ps[bk][:, :], op0=ALU.add, op1=ALU.mult),
                        reads=[B_thg, psb[bk]], writes=[B_ctmp[ci]], cost=0.63)
                    dve(lambda e, dc=dc, ci=ci: e.tensor_tensor(out=mixT[:, dc, :], in0=mixT[:, dc, :], in1=ctmp[ci][:],
                                                                op=ALU.add), reads=[B_ctmp[ci]], writes=[B_mixT], cost=0.63)
            ckpt(13)
            slots = [load_slab2(18), load_slab2(19)]
            xh = (lnm, lnr)
            xhb = (B_lnm, B_lnr)
            for t in range(4):
                r0 = tok0 + t * 128
                for j in range(2):
                    P.dma("sp", "xr%d" % j, lambda e, j=j, r0=r0: e.dma_start(out=xh[j][:, :], in_=x_d[r0:r0 + 128, j * 512:(j + 1) * 512]),
                          writes=[xhb[j]])
                    bk = gbank()

                    def mm(e, slot=slots[j], t=t, bk=bk):
                        ins = None
                        for c in range(8):
                            ins = e.matmul(out=ps[bk][:, :], lhsT=mixT[:, c, t * 128:(t + 1) * 128], rhs=ring[slot][:, c, :],
                                           start=(c == 0), stop=(c == 7))
                        return ins
                    pe(mm, reads=[B_mixT] + ring_bufs(slots[j]), writes=[psb[bk]], cost=2.3)
                    dve(lambda e, bk=bk, j=j: e.scalar_tensor_tensor(
                        out=xh[j][:, :], in0=ps[bk][:, :], scalar=0.25, in1=xh[j][:, :], op0=ALU.mult, op1=ALU.add),
                        reads=[psb[bk]], writes=[xhb[j]], cost=0.63)
                    P.dma("sp", "hr%d" % j, lambda e, j=j, r0=r0: e.dma_start(out=hres_d[r0:r0 + 128, j * 512:(j + 1) * 512], in_=xh[j][:, :]),
                          reads=[xhb[j]])

    if _os.environ.get("KSPLITBUF"):
        for b_ in [B_sm, B_junk, B_rs, B_rtmp, B_oall] + B_ctmp + B_cbf + (B_ring + B_ring2 if "r" in _os.environ["KSPLITBUF"] else []):
            P.split[id(b_)] = True
    stopped = False
    try:
        meta_pass()
        blks = [(seq, blk) for seq in range(NSEQ) for blk in range(NB)]
        if _os.environ.get("KORDER", "1") == "1":
            block(blks[0][0], blks[0][1], "f1")
            block(blks[0][0], blks[0][1], "f2")
            for g, (seq, blk) in enumerate(blks):
                block(seq, blk, "attn")
                if g + 1 < len(blks):
                    block(blks[g + 1][0], blks[g + 1][1], "f1")
                block(seq, blk, "back")
                if g + 1 < len(blks):
                    block(blks[g + 1][0], blks[g + 1][1], "f2")
        else:
            for seq, blk in blks:
                for part in ("f1", "f2", "attn", "back"):
                    block(seq, blk, part)
    except _Stop:
        stopped = True

    fin = [P.last[k] for k in ("hr0", "hr1") if k in P.last]
    if stop_phase1 or stopped:
        P.flush(final_waits=fin)
        free_to(0)
        for c in reversed(pcm):
            c.__exit__(None, None, None)
        P.close()
        return nc
    P.flush(final_waits=fin)
    free_to(n_const)

    NTA = NSEQ * NT
    BLK = 512
    NBLK = (NTOK * 2) // BLK + NE
    NSLOT = NBLK * BLK
    h2_d = nc.dram_tensor("h2s", [NTOK, D], BF16, kind="Internal").ap()
    ys_d = nc.dram_tensor("ys", [NSLOT, D], BF16, kind="Internal").ap()
    st_d = nc.dram_tensor("slot_tok", [NSLOT, 16], I32, kind="Internal").ap()

    g2gf = sb("g2gf", (128, 2 * D), F32)
    junk = sb("junk", (128, 1024), BF16)
    ustr = sb("ustr", (128, 128), F32)
    ustr_b = sb("ustr_b", (128, 128), BF16)
    one_b = sb("one_b", (128, 128), BF16)
    ld(g2gf[:], gvec_d[:, D:3 * D], key="c_ld2")
    ld(ustr[:], ustr_d, key="c_ld2")
    g2 = g2gf[:, 0:D]
    gf = g2gf[:, D:2 * D]
    dve(lambda e: e.tensor_copy(out=ustr_b[:], in_=ustr[:]), reads=C, writes=C)
    dve(lambda e: e.memset(one_b[:], 1.0), writes=C)

    S1 = sb("S1", (128, NTA, NE), F32)
    S2 = sb("S2", (128, NTA, NE), F32)
    RK = sb("RK", (128, NTA, NE), F32)
    GA = sb("GA", (128, NTA, 2), F32)
    DSf = sb("DSf", (128, NTA, 2), F32)
    DSi = sb("DSi", (128, NTA, 2), I32)
    TOK = sb("TOK", (128, max(NTA, 8), 16), I32)
    cum = sb("cum", (128, NE), F32)
    misc = sb("misc", (128, 8, NE), F32)
    misci = sb("misci", (128, 2, NE), I32)
    BE = sb("BE", (128, NBLK), F32)
    IDX = sb("IDX", (128, NBLK, 2), I32)
    IDf = sb("IDf", (128, NBLK, 2), F32)
    base2 = sb("base2", (128, 2), F32)
    base2i = sb("base2i", (128, 2), I32)
    ZQ = NSLOT * 16 // 128
    ZW = min(ZQ, 1024)
    zt = sb("zt", (128, ZW), I32)
    hx = [sb("hx%d" % i, (128, D), F32) for i in range(4)]
    h2f = sb("h2f", (128, D), F32)
    h2b = [sb("h2b%d" % i, (128, D), BF16) for i in range(2)]
    h2Tf = sb("h2Tf", (128, 8, 128), F32)
    eb = sb("eb", (128, NE), BF16)
    sm2s = sb("sm2s", (128, 4, 96), F32)
    SSA = sb("SSA", (128, NTA), F32)
    RSA = sb("RSA", (128, NTA), F32)
    RSy = sb("RSy", (128, NTA), F32)
    RSt = sb("RSt", (128, NTA), F32)
    B_ssa = [Buf() for _ in range(NTA)]
    B_SSA = Buf(); B_RSA = Buf(); B_RSs = Buf()
    rs2 = sb("rs2", (128, 4, 2), F32)
    B_sm2s = [Buf() for _ in range(4)]
    B_rs2 = [Buf() for _ in range(4)]
    SI = [sb("SI%d" % i, (128, 4, 16), I32) for i in range(2)]
    XG = [sb("XG%d" % i, (128, 4, D), BF16) for i in range(2)]
    XT = [sb("XT%d" % i, (128, 8, BLK), BF16) for i in range(2)]
    WG = [sb("WG%d" % i, (128, 8, DE), BF16) for i in range(2)]
    WU = [sb("WU%d" % i, (128, 8, DE), BF16) for i in range(2)]
    WD = [sb("WD%d" % i, (128, 4, D), BF16) for i in range(2)]
    hid = [sb("hid%d" % i, (128, 4, BLK), BF16) for i in range(2)]
    slt = [sb("slt%d" % i, (128, BLK), F32) for i in range(2)]
    YB = [sb("YB%d" % i, (128, D), BF16) for i in range(4)]
    Y1 = [sb("Y1_%d" % i, (128, D), BF16) for i in range(2)]
    Y2 = [sb("Y2_%d" % i, (128, D), BF16) for i in range(2)]
    yo = [sb("yo%d" % i, (128, D), F32) for i in range(2)]
    B_S1 = Buf(); B_S2 = Buf(); B_RK = Buf(); B_GA = Buf(); B_DS = Buf(); B_TOK = Buf(); B_cum = Buf()
    B_misc = Buf(); B_BE = Buf(); B_ID = Buf(); B_zt = Buf()
    B_hx = [Buf() for _ in range(4)]; B_h2f = Buf(); B_h2b = [Buf(), Buf()]; B_h2Tf = Buf(); B_eb = Buf()
    B_SI = [Buf(), Buf()]; B_XG = [Buf(), Buf()]; B_XT = [Buf(), Buf()]
    B_WG = [Buf(), Buf()]; B_WU = [Buf(), Buf()]; B_WD = [Buf(), Buf()]
    B_hid = [Buf(), Buf()]; B_slt = [Buf(), Buf()]; B_YB = [Buf() for _ in range(4)]
    B_Y1 = [Buf(), Buf()]; B_Y2 = [Buf(), Buf()]; B_yo = [Buf(), Buf()]
    B_h2d = Buf(); B_std = Buf(); B_ysd = Buf()
    st2 = {"g": 0, "slt": 0, "yb": 0, "hx": 0}

    def gb2():
        b = st2["g"] % 8
        st2["g"] += 1
        return b

    def pool(fn, reads=(), writes=(), extra=(), cost=None):
        return P.op("pool", fn, reads, writes, extra, cost)

    IOA = bass.IndirectOffsetOnAxis
    BIG = 1.0e30
    pool(lambda e: e.iota(TOK[:], pattern=[[128, max(NTA, 8)], [0, 16]], base=0, channel_multiplier=1), writes=[B_TOK])
    pool(lambda e: e.iota(base2i[:], pattern=[[1, 2]], base=0, channel_multiplier=2), writes=[B_TOK])
    dve(lambda e: e.tensor_copy(out=base2[:], in_=base2i[:]), reads=[B_TOK], writes=C)
    dve(lambda e: e.memset(zt[:], 0), writes=[B_zt])
    dve(lambda e: e.memset(cum[:], 0.0), writes=[B_cum])
    for z0 in range(0, ZQ, ZW):
        zw = min(ZW, ZQ - z0)
        P.dma("sp", "zt", lambda e, z0=z0, zw=zw: e.dma_start(
            out=st_d.rearrange("(p q) c -> p (q c)", p=128)[:, z0:z0 + zw], in_=zt[:, 0:zw]),
            reads=[B_zt], writes=[B_std])

    def tile2a(t):
        r0 = t * 128
        hi = t % 2
        sm2 = sm2s[:, t % 4, :]
        B_sm2 = B_sm2s[t % 4]
        rscr = (rs2[:, t % 4, 0:1], rs2[:, t % 4, 1:2], [B_rs2[t % 4]])
        hq = t % 4
        P.dma("sp", "hx%d" % hq, lambda e, hq=hq, r0=r0: e.dma_start(out=hx[hq][:], in_=hres_d[r0:r0 + 128, :]),
              writes=[B_hx[hq]])
        dve(lambda e, hq=hq, t=t: e.scalar_tensor_tensor(out=h2f[:], in0=hx[hq][:], scalar=RSA[:, t:t + 1], in1=g2,
                                                         op0=ALU.mult, op1=ALU.mult),
            reads=[B_hx[hq], B_RSA] + C, writes=[B_h2f], cost=1.2)
        act(lambda e, hi=hi: e.activation(out=h2b[hi][:], in_=h2f[:], func=AF.Copy), reads=[B_h2f], writes=[B_h2b[hi]])
        P.dma("sp", "h2b%d" % hi, lambda e, hi=hi, r0=r0: e.dma_start(out=h2_d[r0:r0 + 128, :], in_=h2b[hi][:]),
              reads=[B_h2b[hi]], writes=[])
        bks = [gb2(), gb2()]
        for q in range(2):
            def tr(e, q=q, bk=bks[q]):
                ins = None
                for c in range(4):
                    ins = e.transpose(out=ps[bk][:, c * 128:(c + 1) * 128],
                                      in_=h2f[:, (q * 4 + c) * 128:(q * 4 + c + 1) * 128], identity=ident_f[:])
                return ins
            pe(tr, reads=[B_h2f] + C, writes=[psb[bks[q]]])
            dve(lambda e, q=q, bk=bks[q]: e.tensor_copy(out=h2Tf[:, q * 4:(q + 1) * 4, :],
                                                        in_=ps[bk][:, :].rearrange("p (c t) -> p c t", c=4)),
                reads=[psb[bks[q]]], writes=[B_h2Tf])
        bk = gb2()

        def rmm(e, bk=bk):
            ins = None
            for c in range(8):
                ins = e.matmul(out=ps[bk][:, 0:36], lhsT=h2Tf[:, c, :], rhs=wr[:, c, :], start=(c == 0), stop=(c == 7))
            return ins
        pe(rmm, reads=[B_h2Tf] + C, writes=[psb[bk]])
        S2_ = [B_sm2]
        lg = sm2[:, 8:44]
        dve(lambda e, bk=bk: e.tensor_tensor(out=lg, in0=ps[bk][:, 0:36], in1=br[:], op=ALU.add),
            reads=[psb[bk]] + C, writes=S2_)
        dve(lambda e: e.tensor_reduce(out=sm2[:, 3:4], in_=sm2[:, 8:12], axis=AX.X, op=ALU.max), reads=S2_, writes=S2_)
        dve(lambda e: e.tensor_scalar(out=sm2[:, 4:5], in0=sm2[:, 3:4], scalar1=-1.0, scalar2=None, op0=ALU.mult),
            reads=S2_, writes=S2_)
        act(lambda e: e.activation(out=sm2[:, 44:48], in_=sm2[:, 8:12], func=AF.Exp, bias=sm2[:, 4:5],
                                   accum_out=sm2[:, 5:6]), reads=S2_, writes=S2_)
        dve(lambda e: e.reciprocal(out=sm2[:, 6:7], in_=sm2[:, 5:6]), reads=S2_, writes=S2_)
        dve(lambda e: e.tensor_scalar(out=sm2[:, 44:48], in0=sm2[:, 8:12], scalar1=sm2[:, 3:4], scalar2=None,
                                      op0=ALU.is_equal), reads=S2_, writes=S2_)
        dve(lambda e: e.tensor_scalar(out=sm2[:, 44:48], in0=sm2[:, 44:48], scalar1=BIG, scalar2=-BIG,
                                      op0=ALU.mult, op1=ALU.add), reads=S2_, writes=S2_)
        dve(lambda e: e.tensor_tensor(out=sm2[:, 48:80].rearrange("p (a b) -> p a b", a=4),
                                      in0=sm2[:, 12:44].rearrange("p (a b) -> p a b", a=4),
                                      in1=sm2[:, 44:48].unsqueeze(2).to_broadcast([128, 4, 8]), op=ALU.add),
            reads=S2_, writes=S2_)
        dve(lambda e: e.max(out=sm2[:, 80:88], in_=sm2[:, 48:80]), reads=S2_, writes=S2_)
        dve(lambda e: e.tensor_tensor(out=sm2[:, 88:89], in0=sm2[:, 81:82], in1=sm2[:, 80:81], op=ALU.subtract),
            reads=S2_, writes=S2_)
        act(lambda e: e.activation(out=sm2[:, 89:90], in_=sm2[:, 88:89], func=AF.Exp), reads=S2_, writes=S2_)
        dve(lambda e: e.tensor_scalar(out=sm2[:, 90:91], in0=sm2[:, 89:90], scalar1=1.0, scalar2=None, op0=ALU.add),
            reads=S2_, writes=S2_)
        dve(lambda e: e.reciprocal(out=sm2[:, 91:92], in_=sm2[:, 90:91]), reads=S2_, writes=S2_)
        dve(lambda e, t=t: e.tensor_tensor(out=GA[:, t, 0:1], in0=sm2[:, 91:92], in1=sm2[:, 6:7], op=ALU.mult),
            reads=S2_, writes=[B_GA])
        dve(lambda e, t=t: e.tensor_tensor(out=GA[:, t, 1:2], in0=GA[:, t, 0:1], in1=sm2[:, 89:90], op=ALU.mult),
            reads=S2_, writes=[B_GA])
        dve(lambda e, t=t: e.tensor_scalar(out=S1[:, t, :], in0=sm2[:, 48:80], scalar1=sm2[:, 80:81], scalar2=None,
                                           op0=ALU.is_equal), reads=S2_, writes=[B_S1])
        dve(lambda e, t=t: e.tensor_scalar(out=S2[:, t, :], in0=sm2[:, 48:80], scalar1=sm2[:, 81:82], scalar2=None,
                                           op0=ALU.is_equal), reads=S2_, writes=[B_S2])
        dve(lambda e, t=t: e.tensor_tensor(out=eb[:], in0=S1[:, t, :], in1=S2[:, t, :], op=ALU.add),
            reads=[B_S1, B_S2], writes=[B_eb])
        bk2 = gb2()

        def rkmm(e, bk2=bk2):
            e.matmul(out=ps[bk2][:, 0:NE], lhsT=ustr_b[:], rhs=eb[:], start=True, stop=True)
            return e.matmul(out=ps[bk2][:, 64:64 + NE], lhsT=one_b[:], rhs=eb[:], start=True, stop=True)
        pe(rkmm, reads=[B_eb] + C, writes=[psb[bk2]])
        dve(lambda e, t=t, bk2=bk2: e.tensor_tensor(out=RK[:, t, :], in0=ps[bk2][:, 0:NE], in1=cum[:], op=ALU.add),
            reads=[psb[bk2], B_cum], writes=[B_RK])
        dve(lambda e, bk2=bk2: e.tensor_tensor(out=cum[:], in0=ps[bk2][:, 64:64 + NE], in1=cum[:], op=ALU.add),
            reads=[psb[bk2]], writes=[B_cum])

    for t in range(NTA):
        hq = t % 4
        r0 = t * 128
        P.dma("sp", "hx%d" % hq, lambda e, hq=hq, r0=r0: e.dma_start(out=hx[hq][:], in_=hres_d[r0:r0 + 128, :]),
              writes=[B_hx[hq]])
        act(lambda e, hq=hq, t=t: e.activation(out=junk[:, :], in_=hx[hq][:], func=AF.Square, accum_out=SSA[:, t:t + 1]),
            reads=[B_hx[hq]], writes=[B_junk, B_ssa[t]], cost=0.95)
    dve(lambda e: e.tensor_scalar(out=SSA[:], in0=SSA[:], scalar1=1.0 / D, scalar2=EPS, op0=ALU.mult, op1=ALU.add),
        reads=B_ssa, writes=[B_SSA])
    rsqrt(B_SSA, SSA[:], B_RSA, RSA[:], 128, NTA, scr=(RSy[:], RSt[:], [B_RSs]))
    for t in range(NTA):
        tile2a(t)

    M = [B_misc]
    cnt_i = misci[:, 0, :]
    pc_i = misci[:, 1, :]
    dve(lambda e: e.tensor_copy(out=cnt_i, in_=cum[:]), reads=[B_cum], writes=M)
    dve(lambda e: e.tensor_scalar(out=cnt_i, in0=cnt_i, scalar1=float(BLK - 1), scalar2=None, op0=ALU.add), reads=M, writes=M)
    dve(lambda e: e.tensor_scalar(out=pc_i, in0=cnt_i, scalar1=9, scalar2=None, op0=ALU.arith_shift_right), reads=M, writes=M)
    dve(lambda e: e.tensor_scalar(out=pc_i, in0=pc_i, scalar1=9, scalar2=None, op0=ALU.logical_shift_left), reads=M, writes=M)
    dve(lambda e: e.tensor_copy(out=misc[:, 0, :], in_=pc_i), reads=M, writes=M)
    dve(lambda e: e.memset(misc[:, 1, :], 0.0), writes=M)
    dve(lambda e: e.tensor_tensor_scan(out=misc[:, 2, :], data0=misc[:, 0, :], data1=misc[:, 1, :], initial=0.0,
                                       op0=ALU.add, op1=ALU.add), reads=M, writes=M)
    dve(lambda e: e.tensor_tensor(out=misc[:, 3, :], in0=misc[:, 2, :], in1=misc[:, 0, :], op=ALU.subtract),
        reads=M, writes=M)
    dve(lambda e: e.tensor_tensor(out=RK[:], in0=RK[:], in1=misc[:, 3, :].unsqueeze(1).to_broadcast([128, NTA, NE]),
                                  op=ALU.add), reads=M, writes=[B_RK])
    dve(lambda e: e.tensor_tensor(out=S1[:], in0=S1[:], in1=RK[:], op=ALU.mult), reads=[B_RK], writes=[B_S1])
    dve(lambda e: e.tensor_tensor(out=S2[:], in0=S2[:], in1=RK[:], op=ALU.mult), reads=[B_RK], writes=[B_S2])
    dve(lambda e: e.tensor_reduce(out=DSf[:, :, 0], in_=S1[:], axis=AX.X, op=ALU.add), reads=[B_S1], writes=[B_DS])
    dve(lambda e: e.tensor_reduce(out=DSf[:, :, 1], in_=S2[:], axis=AX.X, op=ALU.add), reads=[B_S2], writes=[B_DS])
    dve(lambda e: e.tensor_copy(out=DSi[:], in_=DSf[:]), reads=[B_DS], writes=[B_DS])
    for b in range(NBLK):
        dve(lambda e, b=b: e.tensor_scalar(out=misc[:, 4, :], in0=misc[:, 2, :], scalar1=float(b * BLK), scalar2=None,
                                           op0=ALU.is_le, op1=ALU.add, accum_out=BE[:, b:b + 1]),
            reads=M, writes=M + [B_BE])
    dve(lambda e: e.tensor_scalar(out=BE[:], in0=BE[:], scalar1=float(NE - 1), scalar2=None, op0=ALU.min),
        reads=[B_BE], writes=[B_BE])
    dve(lambda e: e.tensor_scalar(out=IDf[:], in0=BE[:].unsqueeze(2).to_broadcast([128, NBLK, 2]), scalar1=256.0,
                                  scalar2=None, op0=ALU.mult), reads=[B_BE], writes=[B_ID])
    dve(lambda e: e.tensor_tensor(out=IDf[:], in0=IDf[:], in1=base2[:].unsqueeze(1).to_broadcast([128, NBLK, 2]),
                                  op=ALU.add), reads=C, writes=[B_ID])
    dve(lambda e: e.tensor_copy(out=IDX[:], in_=IDf[:]), reads=[B_ID], writes=[B_ID])
    sc_ids = []
    for t in range(NTA):
        for k in range(2):
            sc_ids.append(None)
            sc_ids[-1] = P.dma("pool", "sc%d" % ((2 * t + k) % 8),
                  lambda e, t=t, k=k: e.indirect_dma_start(out=st_d, out_offset=IOA(ap=DSi[:, t, k:k + 1], axis=0),
                                                           in_=TOK[:, t, :], in_offset=None),
                  reads=[B_DS, B_TOK, B_std])
    sc_done = list(sc_ids)
    h2_done = [P.last["h2b%d" % i] for i in range(2)]

    for b in range(NBLK):
        wi = b % 2
        P.dma("sp", "si%d" % wi, lambda e, b=b, wi=wi: e.dma_start(
            out=SI[wi][:], in_=st_d[b * BLK:(b + 1) * BLK, :].rearrange("(j p) c -> p j c", p=128)),
            writes=[B_SI[wi]], extra=sc_done)
        for j in range(4):
            P.dma("pool", "xg%d_%d" % (wi, j), lambda e, wi=wi, j=j: e.indirect_dma_start(
                out=XG[wi][:, j, :], out_offset=None, in_=h2_d, in_offset=IOA(ap=SI[wi][:, j, 0:1], axis=0)),
                reads=[B_SI[wi]], writes=[B_XG[wi]] if j == 0 else [], extra=h2_done + ([B_XG[wi].w] if j else []))
        xg_tok = [P.last["xg%d_%d" % (wi, j)] for j in range(4)]
        fw = {}
        for h_ in range(2):
            for nm, Wt, src, Bw in (("wg", WG, weg_f, B_WG), ("wu", WU, weu_f, B_WU), ("wd", WD, wed_f, B_WD)):
                nc_ = Wt[wi].shape[1] // 2
                t_ = P.dma("pool", "%s%d_%d" % (nm, wi, h_), lambda e, Wt=Wt, src=src, wi=wi, h_=h_, b=b, nc_=nc_:
                           e.indirect_dma_start(out=Wt[wi][:, h_ * nc_:(h_ + 1) * nc_, :].rearrange("p c n -> p (c n)"),
                                                out_offset=None, in_=src, in_offset=IOA(ap=IDX[:, b, h_:h_ + 1], axis=0)),
                           reads=[B_ID], writes=[Bw[wi]] if h_ == 0 else [], extra=[fw.get(nm)], cost=1.5, lat=10.0)
                fw.setdefault(nm, t_)
        wg_tok = [P.last["wg%d_%d" % (wi, c)] for c in range(2)]
        wu_tok = [P.last["wu%d_%d" % (wi, c)] for c in range(2)]
        wd_tok = [P.last["wd%d_%d" % (wi, c)] for c in range(2)]
        for j in range(4):
            bk = gb2()
            pbf = ps[bk][:].bitcast(BF16)

            def tr(e, j=j, pbf=pbf, wi=wi):
                ins = None
                for c in range(8):
                    ins = e.transpose(out=pbf[:, c * 128:(c + 1) * 128], in_=XG[wi][:, j, c * 128:(c + 1) * 128],
                                      identity=ident_b[:])
                return ins
            pe(tr, reads=[B_XG[wi]] + C, writes=[psb[bk]], extra=xg_tok)
            act(lambda e, j=j, pbf=pbf, wi=wi: e.activation(out=XT[wi][:, :, j * 128:(j + 1) * 128],
                                                          in_=pbf.rearrange("p (c t) -> p c t", c=8), func=AF.Copy),
                reads=[psb[bk]], writes=[B_XT[wi]])
        hi = b % 2
        for hc in range(4):
            bg = gb2()
            bu = gb2()

            def mmg(e, W=WG[wi], bk=bg, hc=hc, wi=wi):
                ins = None
                for c in range(8):
                    ins = e.matmul(out=ps[bk][:, :], lhsT=W[:, c, hc * 128:(hc + 1) * 128], rhs=XT[wi][:, c, :],
                                   start=(c == 0), stop=(c == 7))
                return ins
            pe(mmg, reads=[B_XT[wi], B_WG[wi]], writes=[psb[bg]], extra=wg_tok, cost=2.3)

            def mmu(e, W=WU[wi], bk=bu, hc=hc, wi=wi):
                ins = None
                for c in range(8):
                    ins = e.matmul(out=ps[bk][:, :], lhsT=W[:, c, hc * 128:(hc + 1) * 128], rhs=XT[wi][:, c, :],
                                   start=(c == 0), stop=(c == 7))
                return ins
            pe(mmu, reads=[B_XT[wi], B_WU[wi]], writes=[psb[bu]], extra=wu_tok, cost=2.3)
            si_ = st2["slt"] % 2
            st2["slt"] += 1
            act(lambda e, bg=bg, si_=si_: e.activation(out=slt[si_][:], in_=ps[bg][:, :], func=AF.Silu),
                reads=[psb[bg]], writes=[B_slt[si_]], cost=0.6)
            dve(lambda e, bu=bu, si_=si_, hi=hi, hc=hc: e.tensor_tensor(
                out=hid[hi][:, hc, :], in0=slt[si_][:], in1=ps[bu][:, :], op=ALU.mult),
                reads=[B_slt[si_], psb[bu]], writes=[B_hid[hi]], cost=0.63)
        for tt in range(4):
            yi = st2["yb"] % 4
            st2["yb"] += 1
            for j in range(2):
                bk = gb2()

                def mmd(e, W=WD[wi], bk=bk, tt=tt, j=j, hi=hi):
                    ins = None
                    for c in range(4):
                        ins = e.matmul(out=ps[bk][:, :], lhsT=hid[hi][:, c, tt * 128:(tt + 1) * 128],
                                       rhs=W[:, c, j * 512:(j + 1) * 512], start=(c == 0), stop=(c == 3))
                    return ins
                pe(mmd, reads=[B_hid[hi], B_WD[wi]], writes=[psb[bk]], extra=wd_tok, cost=1.15)
                if j == 0:
                    act(lambda e, bk=bk, yi=yi: e.activation(out=YB[yi][:, 0:512], in_=ps[bk][:, :], func=AF.Copy),
                        reads=[psb[bk]], writes=[B_YB[yi]])
                else:
                    dve(lambda e, bk=bk, yi=yi: e.tensor_copy(out=YB[yi][:, 512:1024], in_=ps[bk][:, :]),
                        reads=[psb[bk]], writes=[B_YB[yi]])
            s0 = b * BLK + tt * 128
            P.dma("sp", "yb%d" % yi, lambda e, yi=yi, s0=s0: e.dma_start(out=ys_d[s0:s0 + 128, :], in_=YB[yi][:]),
                  reads=[B_YB[yi]])
    ys_done = [P.last["yb%d" % i] for i in range(4)]

    def tile2c(t):
        r0 = t * 128
        hi = t % 2
        hq = t % 4
        sm2 = sm2s[:, t % 4, :]
        B_sm2 = B_sm2s[t % 4]
        rscr = (rs2[:, t % 4, 0:1], rs2[:, t % 4, 1:2], [B_rs2[t % 4]])
        P.dma("sp", "hx%d" % hq, lambda e, hq=hq, r0=r0: e.dma_start(out=hx[hq][:], in_=hres_d[r0:r0 + 128, :]),
              writes=[B_hx[hq]])
        P.dma("pool", "y1_%d" % hi, lambda e, hi=hi, t=t: e.indirect_dma_start(
            out=Y1[hi][:], out_offset=None, in_=ys_d, in_offset=IOA(ap=DSi[:, t, 0:1], axis=0)),
            reads=[B_DS], writes=[B_Y1[hi]], extra=ys_done)
        P.dma("pool", "y2_%d" % hi, lambda e, hi=hi, t=t: e.indirect_dma_start(
            out=Y2[hi][:], out_offset=None, in_=ys_d, in_offset=IOA(ap=DSi[:, t, 1:2], axis=0)),
            reads=[B_DS], writes=[B_Y2[hi]], extra=ys_done)
        dve(lambda e, hi=hi, hq=hq, t=t: e.scalar_tensor_tensor(out=hx[hq][:], in0=Y1[hi][:], scalar=GA[:, t, 0:1], in1=hx[hq][:],
                                                         op0=ALU.mult, op1=ALU.add),
            reads=[B_Y1[hi], B_GA], writes=[B_hx[hq]])
        dve(lambda e, hi=hi, hq=hq, t=t: e.scalar_tensor_tensor(out=hx[hq][:], in0=Y2[hi][:], scalar=GA[:, t, 1:2], in1=hx[hq][:],
                                                         op0=ALU.mult, op1=ALU.add),
            reads=[B_Y2[hi], B_GA], writes=[B_hx[hq]])
        act(lambda e, hq=hq: e.activation(out=junk[:, :], in_=hx[hq][:], func=AF.Square, accum_out=sm2[:, 0:1]),
            reads=[B_hx[hq]], writes=[B_junk, B_sm2])
        dve(lambda e: e.tensor_scalar(out=sm2[:, 1:2], in0=sm2[:, 0:1], scalar1=1.0 / D, scalar2=EPS,
                                      op0=ALU.mult, op1=ALU.add), reads=[B_sm2], writes=[B_sm2])
        rsqrt(B_sm2, sm2[:, 1:2], B_sm2, sm2[:, 2:3], 128, 1, scr=rscr)
        dve(lambda e, hi=hi, hq=hq: e.scalar_tensor_tensor(out=yo[hi][:], in0=hx[hq][:], scalar=sm2[:, 2:3], in1=gf,
                                                    op0=ALU.mult, op1=ALU.mult),
            reads=[B_hx[hq], B_sm2] + C, writes=[B_yo[hi]])
        P.dma("sp", "yo%d" % hi, lambda e, hi=hi, r0=r0: e.dma_start(out=y_d[r0:r0 + 128, :], in_=yo[hi][:]),
              reads=[B_yo[hi]])

    for t in range(NTA):
        tile2c(t)

    fin = [P.last["yo%d" % i] for i in range(2)]
    P.flush(final_waits=fin)
    free_to(0)
    for c in reversed(pcm):
        c.__exit__(None, None, None)
    P.close()
    return nc


def _host_inputs(inputs, NSEQ, S):
    f = lambda a: np.ascontiguousarray(np.asarray(a, dtype=np.float32))
    w_in = f(inputs["w_in"])[0]
    q_, k_, v_ = w_in[:, 0:1024], w_in[:, 1024:2048], w_in[:, 2048:3072]
    ua, ub = w_in[:, 3072:4096], w_in[:, 4096:5120]
    gl = w_in[:, 5120:7168]
    ucat = np.concatenate([np.concatenate([ua[:, 256 * j:256 * (j + 1)], ub[:, 256 * j:256 * (j + 1)]], axis=1)
                           for j in range(4)], axis=1)
    wcat = np.concatenate([q_, k_, v_, ucat, gl, f(inputs["w_o_attn"])[0], f(inputs["w_pw2"])[0],
                           f(inputs["w_out"])[0]], axis=1)
    bc = lambda v, n=128: np.ascontiguousarray(np.broadcast_to(np.asarray(v, np.float32).reshape(1, -1), (n, np.size(v))))
    gvec = np.concatenate([bc(inputs["norm1_g"][0]), bc(inputs["norm2_g"][0]), bc(inputs["final_g"])], axis=1)
    L = NMETA + S
    half = 8
    inv_freq = 500000.0 ** (-np.arange(0, 16, 2, dtype=np.float32) / 16.0)
    ang = np.arange(L, dtype=np.float32)[:, None] * inv_freq[None, :].astype(np.float32)
    rope = np.concatenate([np.cos(ang), np.sin(ang)], axis=1).astype(np.float32)
    shared = {
        "meta": f(inputs["meta_tokens"]),
        "wcat": np.ascontiguousarray(wcat),
        "ident": np.eye(128, dtype=np.float32),
        "ustr": np.ascontiguousarray(np.triu(np.ones((128, 128), dtype=np.float32), k=1)),
        "gvec": np.ascontiguousarray(gvec),
        "rope": np.ascontiguousarray(rope),
        "convw": np.ascontiguousarray(f(inputs["conv_w"])[0].T),
        "cvec": np.ascontiguousarray(np.stack([f(inputs["conv_b"])[0], f(inputs["conv_ln_g"])[0],
                                               f(inputs["conv_ln_b"])[0]], axis=1)),
        "lamv": np.ascontiguousarray(np.concatenate([bc(inputs["lam_q1"][0]), bc(inputs["lam_k1"][0]),
                                                     bc(inputs["lam_q2"][0]), bc(inputs["lam_k2"][0])], axis=1)),
        "subg": bc(inputs["subln_g"][0]),
        "wr": np.ascontiguousarray(np.concatenate([f(inputs["w_group"])[0], f(inputs["w_router"])[0]], axis=1)),
        "br": np.ascontiguousarray(np.concatenate([bc(inputs["b_group"][0]), bc(inputs["b_router"][0])], axis=1)),
        "weg": np.ascontiguousarray(f(inputs["w_e_gate"])[0].reshape(NE, 8, 128, DE).transpose(0, 2, 1, 3)).reshape(NE * 256, 2048),
        "weu": np.ascontiguousarray(f(inputs["w_e_up"])[0].reshape(NE, 8, 128, DE).transpose(0, 2, 1, 3)).reshape(NE * 256, 2048),
        "wed": np.ascontiguousarray(f(inputs["w_e_down"])[0].reshape(NE, 4, 128, D).transpose(0, 2, 1, 3)).reshape(NE * 256, 2048),
    }
    return shared


def kernel(**inputs):
    x = np.asarray(inputs["x"], dtype=np.float32)
    B, S, _ = x.shape
    ncores = NCORES if B % NCORES == 0 else B
    NSEQ = B // ncores
    nc = build_nc(NSEQ, S)
    shared = _host_inputs(inputs, NSEQ, S)
    in_maps = []
    for c in range(ncores):
        m = dict(shared)
        m["x"] = np.ascontiguousarray(x[c * NSEQ:(c + 1) * NSEQ].reshape(NSEQ * S, D))
        in_maps.append(m)
    res = run_bass_kernel_spmd(nc, in_maps, core_ids=list(range(ncores)))
    out = np.stack([np.asarray(r["y"], dtype=np.float32).reshape(NSEQ, S, D) for r in res.results], axis=0)
    return out.reshape(B, S, D)
```
